# Optimizing a Trainium2 kernel written in Bass

```python
import math
import jax, jax.numpy as jnp
from jax import lax
import numpy as np

D_MODEL = 1024
BATCH = 8
SEQ = 4096
DEPTH = 1

CHUNK = 64
Q_BLOCK = 128
N_DIFF_HEADS = 4
DIFF_HEAD_DIM = 64
ATTN_WIDTH = N_DIFF_HEADS * 2 * DIFF_HEAD_DIM
SSM_WIDTH = D_MODEL - ATTN_WIDTH
SSM_GROUP_CH = 16
SSM_GROUPS = SSM_WIDTH // SSM_GROUP_CH
SSM_STATE = 64
MIX_WIDTH = ATTN_WIDTH + SSM_WIDTH
IN_PROJ_WIDTH = 3 * ATTN_WIDTH + SSM_WIDTH
N_EXPERT_GROUPS = 4
EXPERTS_PER_GROUP = 8
N_EXPERTS = N_EXPERT_GROUPS * EXPERTS_PER_GROUP
INNER_TOP_K = 2
D_EXPERT = D_MODEL // 2
MOE_BLOCK = 128
LN_EPS = 1e-5
RMS_EPS = 1e-5

kernel_name = "hymba_style_diffattn_s5_hiermoe_deepnorm"


def _layernorm(x, g, b):
    xf = x.astype(jnp.float32)
    mu = jnp.mean(xf, axis=-1, keepdims=True)
    var = jnp.mean(jnp.square(xf - mu), axis=-1, keepdims=True)
    y = (xf - mu) * lax.rsqrt(var + LN_EPS) * g.astype(jnp.float32) + b.astype(jnp.float32)
    return y.astype(x.dtype)


def _lambda_init(layer_idx):
    return 0.8 - 0.6 * math.exp(-0.3 * layer_idx)


def _diff_attention(q, k, v, lam, lam_init, subln_g):
    b_, s_ = q.shape[0], q.shape[1]
    nb = s_ // Q_BLOCK
    scale = DIFF_HEAD_DIM ** -0.5
    qb = q.reshape(b_, nb, Q_BLOCK, N_DIFF_HEADS, 2, DIFF_HEAD_DIM).swapaxes(0, 1)
    k_chunk = jnp.arange(s_) // CHUNK

    def block(args):
        qi, bi = args
        sc = jnp.einsum('bqhcd,bkhcd->bhcqk', qi, k).astype(jnp.float32) * scale
        q_chunk = (bi * Q_BLOCK + jnp.arange(Q_BLOCK)) // CHUNK
        mask = k_chunk[None, :] <= q_chunk[:, None]
        sc = jnp.where(mask, sc, -jnp.inf)
        p = jax.nn.softmax(sc, axis=-1)
        attn = p[:, :, 0] - lam * p[:, :, 1]
        return jnp.einsum('bhqk,bkhe->bqhe', attn.astype(v.dtype), v)

    o = lax.map(block, (qb, jnp.arange(nb)))
    o = o.swapaxes(0, 1).reshape(b_, s_, N_DIFF_HEADS, 2 * DIFF_HEAD_DIM).astype(jnp.float32)
    o = o * lax.rsqrt(jnp.mean(jnp.square(o), axis=-1, keepdims=True) + RMS_EPS)
    o = o * subln_g.astype(jnp.float32) * (1.0 - lam_init)
    return o.reshape(b_, s_, ATTN_WIDTH).astype(q.dtype)


def _scan_op(e1, e2):
    a1, b1 = e1
    a2, b2 = e2
    return a1 * a2, a2 * b1 + b2


def _s5_mixer(u, a_re, a_im, log_dt, b_re, b_im, c_re, c_im, d, w_glu, b_glu):
    b_, s_ = u.shape[0], u.shape[1]
    nc = s_ // CHUNK
    f32 = jnp.float32
    uf = u.astype(f32).reshape(b_, nc, CHUNK, SSM_GROUPS, SSM_GROUP_CH).swapaxes(0, 1)
    lam = lax.complex(a_re.astype(f32), a_im.astype(f32))
    dt = jnp.exp(log_dt.astype(f32))[:, None]
    lam_bar = jnp.exp(lam * dt)
    b_bar = ((lam_bar - 1.0) / lam)[..., None] * lax.complex(b_re.astype(f32), b_im.astype(f32))
    c = lax.complex(c_re.astype(f32), c_im.astype(f32))
    dd = d.astype(f32)

    def step(h_prev, u_c):
        bu = jnp.einsum('gpc,bsgc->bsgp', b_bar, u_c.astype(jnp.complex64))
        a = jnp.broadcast_to(lam_bar, bu.shape)
        a_cum, h_loc = lax.associative_scan(_scan_op, (a, bu), axis=1)
        h = a_cum * h_prev[:, None] + h_loc
        y = jnp.real(jnp.einsum('gcp,bsgp->bsgc', c, h)) + dd * u_c
        return h[:, -1], y

    h0 = jnp.zeros((b_, SSM_GROUPS, SSM_STATE), jnp.complex64)
    _, y = lax.scan(step, h0, uf)
    y = y.swapaxes(0, 1).reshape(b_, s_, SSM_WIDTH)
    z = jax.nn.gelu(y)
    z = z * jax.nn.sigmoid(z @ w_glu.astype(f32) + b_glu.astype(f32))
    return z.astype(u.dtype)


def _hier_moe(h2d, w_rg, w_re, w_gate, w_up, w_down):
    t = h2d.shape[0]
    tk = t * INNER_TOP_K
    g_prob = jax.nn.softmax((h2d @ w_rg).astype(jnp.float32), axis=-1)
    g_p, g_idx = lax.top_k(g_prob, 1)
    e_logits = (h2d @ w_re).astype(jnp.float32).reshape(t, N_EXPERT_GROUPS, EXPERTS_PER_GROUP)
    e_logits = jnp.take_along_axis(e_logits, g_idx[:, :, None], axis=1)[:, 0]
    e_prob = jax.nn.softmax(e_logits, axis=-1)
    p2, i2 = lax.top_k(e_prob, INNER_TOP_K)
    p2 = p2 / jnp.sum(p2, axis=-1, keepdims=True)
    expert = g_idx * EXPERTS_PER_GROUP + i2
    weight = g_p * p2

    flat_e = expert.reshape(-1)
    flat_tok = jnp.repeat(jnp.arange(t, dtype=jnp.int32), INNER_TOP_K)
    flat_w = weight.reshape(-1)
    order = jnp.argsort(flat_e, stable=True)
    se = flat_e[order]
    counts = jnp.bincount(flat_e, length=N_EXPERTS)
    starts = jnp.cumsum(counts) - counts
    pcounts = (counts + MOE_BLOCK - 1) // MOE_BLOCK * MOE_BLOCK
    pends = jnp.cumsum(pcounts)
    pstarts = pends - pcounts
    dest = pstarts[se] + jnp.arange(tk) - starts[se]
    n_rows = tk + N_EXPERTS * MOE_BLOCK
    n_blocks = n_rows // MOE_BLOCK
    row_tok = jnp.zeros((n_rows,), jnp.int32).at[dest].set(flat_tok[order])
    row_w = jnp.zeros((n_rows,), jnp.float32).at[dest].set(flat_w[order])
    block_e = jnp.minimum(jnp.searchsorted(pends, jnp.arange(n_blocks) * MOE_BLOCK, side='right'),
                          N_EXPERTS - 1)

    def block_fn(args):
        tok, w, e = args
        xb = h2d[tok]
        hid = jax.nn.silu(xb @ w_gate[e]) * (xb @ w_up[e])
        yb = hid @ w_down[e]
        return yb * w[:, None].astype(yb.dtype)

    ys = lax.map(block_fn, (row_tok.reshape(n_blocks, MOE_BLOCK),
                            row_w.reshape(n_blocks, MOE_BLOCK), block_e))
    out = jnp.zeros_like(h2d).at[row_tok].add(ys.reshape(n_rows, -1).astype(h2d.dtype))
    return out


def setup_inputs(seed: int = 0) -> dict:
    key = jax.random.key(seed)
    ks = jax.random.split(key, 32)
    L = DEPTH
    beta = (8.0 * DEPTH) ** -0.25
    f32 = jnp.float32

    def nrm(k, shape, scale):
        return jax.random.normal(k, shape, f32) * scale

    a_im_base = jnp.pi * jnp.arange(SSM_STATE, dtype=f32)
    return {
        "x": nrm(ks[0], (BATCH, SEQ, D_MODEL), 1.0),
        "w_in": nrm(ks[1], (L, D_MODEL, IN_PROJ_WIDTH), D_MODEL ** -0.5),
        "lam_q1": nrm(ks[2], (L, DIFF_HEAD_DIM), 0.1),
        "lam_k1": nrm(ks[3], (L, DIFF_HEAD_DIM), 0.1),
        "lam_q2": nrm(ks[4], (L, DIFF_HEAD_DIM), 0.1),
        "lam_k2": nrm(ks[5], (L, DIFF_HEAD_DIM), 0.1),
        "subln_g": 1.0 + nrm(ks[6], (L, 2 * DIFF_HEAD_DIM), 0.02),
        "ssm_a_re": -0.5 + nrm(ks[7], (L, SSM_GROUPS, SSM_STATE), 0.01),
        "ssm_a_im": a_im_base + nrm(ks[8], (L, SSM_GROUPS, SSM_STATE), 0.01),
        "ssm_log_dt": jax.random.uniform(ks[9], (L, SSM_GROUPS), f32, math.log(1e-3), math.log(1e-1)),
        "ssm_b_re": nrm(ks[10], (L, SSM_GROUPS, SSM_STATE, SSM_GROUP_CH), (2 * SSM_GROUP_CH) ** -0.5),
        "ssm_b_im": nrm(ks[11], (L, SSM_GROUPS, SSM_STATE, SSM_GROUP_CH), (2 * SSM_GROUP_CH) ** -0.5),
        "ssm_c_re": nrm(ks[12], (L, SSM_GROUPS, SSM_GROUP_CH, SSM_STATE), SSM_STATE ** -0.5),
        "ssm_c_im": nrm(ks[13], (L, SSM_GROUPS, SSM_GROUP_CH, SSM_STATE), SSM_STATE ** -0.5),
        "ssm_d": nrm(ks[14], (L, SSM_GROUPS, SSM_GROUP_CH), 1.0),
        "w_glu": nrm(ks[15], (L, SSM_WIDTH, SSM_WIDTH), SSM_WIDTH ** -0.5),
        "b_glu": nrm(ks[16], (L, SSM_WIDTH), 0.01),
        "w_out": nrm(ks[17], (L, MIX_WIDTH, D_MODEL), MIX_WIDTH ** -0.5 * beta),
        "ln1_g": 1.0 + nrm(ks[18], (L, D_MODEL), 0.02),
        "ln1_b": nrm(ks[19], (L, D_MODEL), 0.01),
        "w_router_group": nrm(ks[20], (L, D_MODEL, N_EXPERT_GROUPS), D_MODEL ** -0.5),
        "w_router_expert": nrm(ks[21], (L, D_MODEL, N_EXPERTS), D_MODEL ** -0.5),
        "w_exp_gate": nrm(ks[22], (L, N_EXPERTS, D_MODEL, D_EXPERT), D_MODEL ** -0.5),
        "w_exp_up": nrm(ks[23], (L, N_EXPERTS, D_MODEL, D_EXPERT), D_MODEL ** -0.5),
        "w_exp_down": nrm(ks[24], (L, N_EXPERTS, D_EXPERT, D_MODEL), D_EXPERT ** -0.5 * beta),
        "ln2_g": 1.0 + nrm(ks[25], (L, D_MODEL), 0.02),
        "ln2_b": nrm(ks[26], (L, D_MODEL), 0.01),
    }


def reference(x, w_in, lam_q1, lam_k1, lam_q2, lam_k2, subln_g, ssm_a_re, ssm_a_im, ssm_log_dt,
              ssm_b_re, ssm_b_im, ssm_c_re, ssm_c_im, ssm_d, w_glu, b_glu, w_out, ln1_g, ln1_b,
              w_router_group, w_router_expert, w_exp_gate, w_exp_up, w_exp_down, ln2_g, ln2_b):
    alpha = (2.0 * DEPTH) ** 0.25
    b_, s_, d_ = x.shape
    for i in range(DEPTH):
        proj = x @ w_in[i]
        q, k, v, u = jnp.split(proj, [ATTN_WIDTH, 2 * ATTN_WIDTH, 3 * ATTN_WIDTH], axis=-1)
        q = q.reshape(b_, s_, N_DIFF_HEADS, 2, DIFF_HEAD_DIM)
        k = k.reshape(b_, s_, N_DIFF_HEADS, 2, DIFF_HEAD_DIM)
        v = v.reshape(b_, s_, N_DIFF_HEADS, 2 * DIFF_HEAD_DIM)
        lam_init = _lambda_init(i)
        lam = (jnp.exp(jnp.sum(lam_q1[i].astype(jnp.float32) * lam_k1[i].astype(jnp.float32)))
               - jnp.exp(jnp.sum(lam_q2[i].astype(jnp.float32) * lam_k2[i].astype(jnp.float32)))
               + lam_init)
        a_out = _diff_attention(q, k, v, lam, lam_init, subln_g[i])
        s_out = _s5_mixer(u, ssm_a_re[i], ssm_a_im[i], ssm_log_dt[i], ssm_b_re[i], ssm_b_im[i],
                          ssm_c_re[i], ssm_c_im[i], ssm_d[i], w_glu[i], b_glu[i])
        mix = jnp.concatenate([a_out, s_out], axis=-1) @ w_out[i]
        h = _layernorm(alpha * x + mix, ln1_g[i], ln1_b[i])
        moe = _hier_moe(h.reshape(b_ * s_, d_), w_router_group[i], w_router_expert[i],
                        w_exp_gate[i], w_exp_up[i], w_exp_down[i]).reshape(b_, s_, d_)
        x = _layernorm(alpha * h + moe, ln2_g[i], ln2_b[i])
    return x
```

```python
import os, math, contextlib
import numpy as np
import concourse.bass as bass
import concourse.mybir as mybir
from concourse.bass_utils import run_bass_kernel_spmd

F32 = mybir.dt.float32; BF16 = mybir.dt.bfloat16; I32 = mybir.dt.int32
AF = mybir.ActivationFunctionType; ALU = mybir.AluOpType; AX = mybir.AxisListType

T = 4096; D = 1024; NT = 32
NEXP = 32; CAP = 384; NSLOT = NEXP * CAP
ALPHA = 2.0 ** 0.25
LAM_INIT = 0.8 - 0.6 * math.exp(0.0)
LN_EPS = 1e-5; RMS_EPS = 1e-5
LCH = 8; NCH = T // LCH


SAME_ENGINE_NOWAIT = set(os.environ.get('MK_NOWAIT', 'pe').split(','))


class Sched:
    def __init__(self, nc, es):
        self.nc = nc; self.es = es
        self.E = {'pe': nc.tensor, 'act': nc.scalar, 'dve': nc.vector, 'pool': nc.gpsimd, 'sp': nc.sync}
        self.sem = {}; self.cnt = {}
        self.waited = {e: {} for e in self.E}
        self.lastw = {}; self.readers = {}
        for e in ('pe', 'act', 'dve', 'pool'):
            self._sem('c_' + e)

    def _sem(self, name):
        if name not in self.sem:
            self.sem[name] = self.es.enter_context(self.nc.semaphore(name))
            self.cnt[name] = 0
        return self.sem[name]

    def _wait(self, eng, toks):
        best = {}
        for (sn, val, peng) in toks:
            if peng == eng and eng in SAME_ENGINE_NOWAIT:
                continue
            if val > best.get(sn, 0):
                best[sn] = val
        for sn, val in best.items():
            if self.waited[eng].get(sn, 0) < val:
                self.E[eng].wait_ge(self.sem[sn], val)
                self.waited[eng][sn] = val

    def _collect(self, r, w, join=False):
        toks = []
        for k in r:
            if k in self.lastw:
                toks.append(self.lastw[k])
        for k in w:
            if k in self.lastw and not join:
                toks.append(self.lastw[k])
            toks.extend(self.readers.get(k, ()))
        return toks

    def _record(self, tok, r, w, join=False):
        for k in r:
            self.readers.setdefault(k, []).append(tok)
        for k in w:
            self.lastw[k] = tok
            if not join:
                self.readers[k] = []

    def op(self, eng, fn, r=(), w=()):
        self._wait(eng, self._collect(r, w))
        ins = fn(self.E[eng])
        sn = 'c_' + eng
        self.cnt[sn] += 1
        ins.then_inc(self.sem[sn], 1)
        self._record((sn, self.cnt[sn], eng), r, w)

    def dma(self, q, fn, sem, r=(), w=(), join=False):
        self._sem(sem)
        self._wait(q, self._collect(r, w, join))
        ins = fn(self.E[q])
        self.cnt[sem] += 16
        ins.then_inc(self.sem[sem], 16)
        self._record((sem, self.cnt[sem], 'dma'), r, w, join)

    def barrier(self):
        for e in self.E:
            toks = [(sn, v, 'x') for sn, v in self.cnt.items() if v > 0]
            self._wait(e, toks)

    def finish(self, keys):
        self._wait('sp', self._collect(keys, ()))


def build_program(stage):
    SKIP = os.environ.get('MK_SKIP', '0') == '1'
    nc = bass.Bass("TRN2", target_bir_lowering=False)
    dr = {}

    def din(name, shape, dt=F32):
        dr[name] = nc.dram_tensor(name, list(shape), dt, kind="ExternalInput").ap()
        return dr[name]

    x = din("x", [T, D]); w_in = din("w_in", [D, 2048])
    lamv = din("lamv", [4, 64]); subg = din("subln_g", [128])
    a_re = din("ssm_a_re", [32, 64]); a_im = din("ssm_a_im", [32, 64]); log_dt = din("ssm_log_dt", [32])
    b_re = din("ssm_b_re", [32, 64, 16]); b_im = din("ssm_b_im", [32, 64, 16])
    c_re = din("ssm_c_re", [32, 16, 64]); c_im = din("ssm_c_im", [32, 16, 64])
    ssm_d = din("ssm_d", [512]); w_glu = din("w_glu", [512, 512]); b_glu = din("b_glu", [512])
    w_out = din("w_out", [D, D]); ln1 = din("ln1", [2, D]); ln2 = din("ln2", [2, D])
    w_r = din("w_r", [D, 36])
    w_g = din("w_exp_gate", [NEXP, D, 512]); w_u = din("w_exp_up", [NEXP, D, 512]); w_d = din("w_exp_down", [NEXP, 512, D])
    cident = din("c_ident", [128, 128]); ctri = din("c_tri", [128, 128]); ciota = din("c_iota", [128, 64])
    cbd = din("c_bdmask", [128, 128]); crm = din("c_rmask", [128, 4])
    s_are = din("s_are", [128, 16]); s_aim = din("s_aim", [128, 16]); s_ldt = din("s_ldt", [128, 16])
    s_bre = din("s_bre", [128, 256]); s_bim = din("s_bim", [128, 256])
    s_cre = din("s_cre", [128, 256]); s_cim = din("s_cim", [128, 256])
    s_d = din("s_d", [128, 4]); s_bglu = din("s_bglu", [128, 4])
    y = nc.dram_tensor("y", [T, D], F32, kind="ExternalOutput").ap()
    dbg = nc.dram_tensor("dbg", [T, D], F32, kind="ExternalOutput").ap() if stage != 'F' else None
    xscr = nc.dram_tensor("xscr", [NSLOT + 128, D], BF16, kind="Internal").ap()
    yscr = nc.dram_tensor("yscr", [NSLOT + 128, D], BF16, kind="Internal").ap()
    hscr = nc.dram_tensor("hscr", [T, D], F32, kind="Internal").ap()

    es = contextlib.ExitStack()
    with es:
        S = Sched(nc, es)
        sb = lambda name, shape, dt, st=es: st.enter_context(nc.sbuf_tensor(name, list(shape), dt))
        psd = [es.enter_context(nc.psum_tensor(f"psd{i}", [128, 1024], F32)) for i in range(4)]
        ps = [psd[i // 2][:, (i % 2) * 512:(i % 2 + 1) * 512] for i in range(8)]
        PK = lambda i: ('psd', i // 2) if i < 4 else ('ps', i)

        ident = sb("ident", [128, 128], F32); identb = sb("identb", [128, 128], BF16)
        onesb = sb("onesb", [128, 128], BF16); onesf = sb("onesf", [128, 128], F32)
        S.dma('sp', lambda e: e.dma_start(out=ident[:], in_=cident[:, :]), 'c0', w=['ident'])
        S.op('dve', lambda e: e.tensor_copy(out=identb[:], in_=ident[:]), r=['ident'], w=['identb'])
        S.op('dve', lambda e: e.memset(onesb[:], 1.0), w=['onesb'])
        S.op('dve', lambda e: e.memset(onesf[:], 1.0), w=['onesf'])

        catA = sb("catA", [128, 4, T], BF16)
        uT = sb("uT", [128, 4, T], BF16)

        esB = contextlib.ExitStack()
        with esB:
            qT = sb("qT", [128, 4, T], BF16, esB); kT = sb("kT", [128, 4, T], BF16, esB)
            V = sb("V", [128, NT, 512], BF16, esB)
            esA = contextlib.ExitStack()
            with esA:
                winb = sb("winb", [128, 8, 2048], BF16, esA)
                xs = [sb(f"xs{i}", [128, 512], F32, esA) for i in range(3)]
                xTb = [sb("xTb0", [128, 8, 512], BF16, esA)]
                for k in range(8):
                    S.dma('pool', lambda e, k=k: e.dma_start(out=winb[:, k, :], in_=w_in[k * 128:(k + 1) * 128, :]),
                          'win', w=[('winb', kk) for kk in range(8)], join=True)
                ev = 0; xi = 0
                for tb in range(0 if SKIP else 8):
                    xt = xTb[0]; xk = ('xTb', 0)
                    for i in range(4):
                        ti = tb * 4 + i
                        for half in range(2):
                            xsi = xs[xi % 3]; xsk = ("xs", xi % 3); xsn = f"xs{xi % 3}"; xi += 1
                            S.dma('sp', lambda e, xsi=xsi, ti=ti, half=half: e.dma_start(
                                out=xsi[:], in_=x[ti * 128:(ti + 1) * 128, half * 512:(half + 1) * 512]), xsn, w=[xsk])
                            pb = (ti % 2) * 2 + half
                            for kk in range(4):
                                S.op('pe', lambda e, pb=pb, kk=kk, xsi=xsi: e.transpose(
                                    out=ps[pb][:, kk * 128:(kk + 1) * 128], in_=xsi[:, kk * 128:(kk + 1) * 128], identity=ident[:]),
                                     r=[xsk, 'ident'], w=[PK(pb)])
                            dst = xt[:, half * 4:half * 4 + 4, i * 128:(i + 1) * 128]
                            src = ps[pb][:, :].rearrange("p (a b) -> p a b", a=4)
                            if half == 0:
                                S.op('act', lambda e, dst=dst, src=src: e.activation(out=dst, in_=src, func=AF.Copy),
                                     r=[PK(pb)], w=[xk])
                            else:
                                S.op('dve', lambda e, dst=dst, src=src: e.tensor_copy(out=dst, in_=src),
                                     r=[PK(pb)], w=[xk])
                    for fc in list(range(0, 8)) + list(range(12, 16)):
                        pb = 4 + (ev % 4); ev += 1
                        for k in range(8):
                            S.op('pe', lambda e, pb=pb, k=k, fc=fc: e.matmul(
                                ps[pb][:, :], lhsT=winb[:, k, fc * 128:(fc + 1) * 128], rhs=xt[:, k, :],
                                start=(k == 0), stop=(k == 7)), r=[('winb', k), xk], w=[PK(pb)])
                        if fc < 4:
                            dst = qT[:, fc, tb * 512:(tb + 1) * 512]; dk = ('qT', fc); sc = 0.125
                        elif fc < 8:
                            dst = kT[:, fc - 4, tb * 512:(tb + 1) * 512]; dk = ('kT', fc - 4); sc = 1.0
                        else:
                            dst = uT[:, fc - 12, :].rearrange("p (s c) -> p s c", s=LCH)[:, :, tb * 64:(tb + 1) * 64]; dk = ('uT', fc - 12); sc = 1.0
                        src = ps[pb][:, :] if fc < 12 else ps[pb][:, :].rearrange("p (c s) -> p s c", s=LCH)
                        if ev % 2 == 0:
                            S.op('act', lambda e, dst=dst, src=src, sc=sc: e.activation(out=dst, in_=src, func=AF.Copy, scale=sc),
                                 r=[PK(pb)], w=[dk])
                        else:
                            S.op('dve', lambda e, dst=dst, src=src, sc=sc: e.tensor_scalar(out=dst, in0=src, scalar1=sc, scalar2=None, op0=ALU.mult),
                                 r=[PK(pb)], w=[dk])
                    for i in range(4):
                        ti = tb * 4 + i
                        pb = 4 + (ev % 4); ev += 1
                        for k in range(8):
                            S.op('pe', lambda e, pb=pb, k=k, i=i: e.matmul(
                                ps[pb][:, :], lhsT=xt[:, k, i * 128:(i + 1) * 128], rhs=winb[:, k, 1024:1536],
                                start=(k == 0), stop=(k == 7)), r=[('winb', k), xk], w=[PK(pb)])
                        if ev % 2 == 0:
                            S.op('act', lambda e, ti=ti, pb=pb: e.activation(out=V[:, ti, :], in_=ps[pb][:, :], func=AF.Copy),
                                 r=[PK(pb)], w=[('V', ti)])
                        else:
                            S.op('dve', lambda e, ti=ti, pb=pb: e.tensor_copy(out=V[:, ti, :], in_=ps[pb][:, :]),
                                 r=[PK(pb)], w=[('V', ti)])
                S.barrier()
                if stage == 'A':
                    return nc
            lamb = sb("lamb", [128, 4, 64], F32, esB)
            lsm = sb("lsm", [128, 8], F32, esB)
            gsc = sb("gsc", [128, 1], F32, esB)
            S.dma('sp', lambda e: e.dma_start(out=lamb[:].rearrange("p a b -> p (a b)"),
                                              in_=lamv.rearrange("a b -> (a b)").partition_broadcast(128)), 'c0', w=['lamb'])
            S.dma('sp', lambda e: e.dma_start(out=gsc[:], in_=subg.rearrange("(p o) -> p o", o=1)), 'c1', w=['gsc'])
            S.op('dve', lambda e: e.tensor_tensor(out=lamb[:, 0, :], in0=lamb[:, 0, :], in1=lamb[:, 1, :], op=ALU.mult), r=['lamb'], w=['lamb'])
            S.op('dve', lambda e: e.tensor_tensor(out=lamb[:, 2, :], in0=lamb[:, 2, :], in1=lamb[:, 3, :], op=ALU.mult), r=['lamb'], w=['lamb'])
            S.op('dve', lambda e: e.reduce_sum(out=lsm[:, 0:1], in_=lamb[:, 0, :], axis=AX.X), r=['lamb'], w=['lsm'])
            S.op('dve', lambda e: e.reduce_sum(out=lsm[:, 1:2], in_=lamb[:, 2, :], axis=AX.X), r=['lamb'], w=['lsm'])
            S.op('act', lambda e: e.activation(out=lsm[:, 0:2], in_=lsm[:, 0:2], func=AF.Exp), r=['lsm'], w=['lsm'])
            S.op('dve', lambda e: e.tensor_tensor(out=lsm[:, 2:3], in0=lsm[:, 1:2], in1=lsm[:, 0:1], op=ALU.subtract), r=['lsm'], w=['lsm'])
            S.op('dve', lambda e: e.tensor_scalar(out=lsm[:, 2:3], in0=lsm[:, 2:3], scalar1=-LAM_INIT, scalar2=None, op0=ALU.add), r=['lsm'], w=['lsm'])
            S.op('dve', lambda e: e.tensor_scalar(out=gsc[:], in0=gsc[:], scalar1=1.0 - LAM_INIT, scalar2=None, op0=ALU.mult), r=['gsc'], w=['gsc'])

            NPT = 4
            PT = [sb(f"PT{i}", [128, 2, 512], BF16, esB) for i in range(NPT)]
            ets = [[sb(f"et{p}{i}", [128, 512], F32, esB) for i in range(4)] for p in range(2)]
            rinv = sb("rinv", [128, 2, 512], F32, esB)
            Oc = sb("Oc", [128, 2, 512], F32, esB)
            racc1 = [sb(f"racc1{p}", [128, 512], F32, esB) for p in range(2)]
            steps = []
            blk = 0
            for h in range(0 if SKIP else 4):
                for qb in range(8):
                    nj = 4 * qb + 4
                    for j in range(nj):
                        steps.append((h, qb, j, nj, blk))
                    blk += 1
            LOOK = 2

            def SD(i):
                return psd[i][:, :].rearrange("p (c f) -> p c f", c=2)

            def emit_scores(n):
                h, qb, j, nj, blk = steps[n]
                m = j - 4 * qb
                c0 = 128 * m if m > 0 else 0
                sd = n % 2; ptk = n % NPT
                for c in range(2):
                    S.op('pe', lambda e, c=c: e.matmul(
                        ps[2 * sd + c][:, c0:512], lhsT=kT[64 * c:64 * c + 64, h, j * 128:(j + 1) * 128],
                        rhs=qT[64 * c:64 * c + 64, h, qb * 512 + c0:(qb + 1) * 512], start=True, stop=True),
                         r=[('qT', h), ('kT', h)], w=[('psd', sd)])
                S.op('act', lambda e: e.activation(out=PT[ptk][:, :, c0:512], in_=SD(sd)[:, :, c0:512], func=AF.Exp),
                     r=[('psd', sd)], w=[('PT', ptk)])
                if m >= 0:
                    S.op('dve', lambda e: e.memset(PT[ptk][64:128, :, c0:c0 + 64], 0.0), r=[], w=[('PT', ptk)])
                ra = racc1[blk % 2]; rk = ('racc1', blk % 2)
                if j == 0:
                    S.op('dve', lambda e: e.tensor_copy(out=ra[:, :], in_=PT[ptk][:, 1, :]), r=[('PT', ptk)], w=[rk])
                else:
                    S.op('dve', lambda e: e.tensor_tensor(out=ra[:, c0:512], in0=ra[:, c0:512], in1=PT[ptk][:, 1, c0:512], op=ALU.add),
                         r=[('PT', ptk), rk], w=[rk])

            def emit_pv(n):
                h, qb, j, nj, blk = steps[n]
                m = j - 4 * qb
                c0 = 128 * m if m > 0 else 0
                ptk = n % NPT
                for c in range(2):
                    S.op('pe', lambda e, c=c: e.matmul(
                        ps[4 + c][:, c0:512], lhsT=V[:, j, h * 128:(h + 1) * 128], rhs=PT[ptk][:, c, c0:512],
                        start=(j == 0), stop=(j == nj - 1)), r=[('PT', ptk), ('V', j)], w=[PK(4 + c)])
                S.op('pe', lambda e: e.matmul(
                    ps[6][:, c0:512], lhsT=onesb[:, :], rhs=PT[ptk][:, 0, c0:512],
                    start=(j == 0), stop=(j == nj - 1)), r=[('PT', ptk), 'onesb'], w=[('psR',)])
                if j == nj - 1:
                    epilogue(h, qb, blk, n)

            def epilogue(h, qb, blk, n):
                par = blk % 2
                e0, e1, e2, e3 = ets[par]
                k1, k2, k3 = ('e1', par), ('e2', par), ('e3', par)
                S.op('dve', lambda e: e.tensor_copy(out=Oc[:, :, :], in_=SD(2)[:, :, :]), r=[PK(4), PK(5)], w=['Oc'])
                S.op('pe', lambda e: e.matmul(ps[7][:, :], lhsT=onesf[:, :], rhs=racc1[par][:, :], start=True, stop=True), r=[('racc1', par), 'onesf'], w=[('psR',)])
                S.op('act', lambda e: e.activation(out=rinv[:, :, :], in_=SD(3)[:, :, :], func=AF.Ln), r=[('psR',)], w=['rinv'])
                S.op('act', lambda e: e.activation(out=rinv[:, :, :], in_=rinv[:, :, :], func=AF.Exp, scale=-1.0), r=['rinv'], w=['rinv'])
                S.op('dve', lambda e: e.tensor_tensor(out=e1[:], in0=Oc[:, 0, :], in1=rinv[:, 0, :], op=ALU.mult), r=['Oc', 'rinv'], w=[k1])
                S.op('dve', lambda e: e.tensor_tensor(out=e2[:], in0=Oc[:, 1, :], in1=rinv[:, 1, :], op=ALU.mult), r=['Oc', 'rinv'], w=[k2])
                S.op('dve', lambda e: e.scalar_tensor_tensor(out=e1[:], in0=e2[:], scalar=lsm[:, 2:3], in1=e1[:], op0=ALU.mult, op1=ALU.add),
                     r=[k1, k2, 'lsm'], w=[k1])
                S.op('pool', lambda e: e.tensor_tensor(out=e3[:], in0=e1[:], in1=e1[:], op=ALU.mult), r=[k1], w=[k3])
                pending.append((n + 4, h, qb, par))

            def epilogue2(h, qb, par, n):
                e0, e1, e2, e3 = ets[par]
                k1, k2, k3 = ('e1', par), ('e2', par), ('e3', par)
                sd = (n + 1) % 2
                S.op('pe', lambda e: e.matmul(ps[2 * sd][:, :], lhsT=onesf[:, :], rhs=e3[:], start=True, stop=True), r=[k3, 'onesf'], w=[('psd', sd)])
                S.op('act', lambda e: e.activation(out=e2[:], in_=ps[2 * sd][:, :], func=AF.Ln, scale=1.0 / 128.0, bias=epsb[:, 0:1]), r=[('psd', sd), 'epsb'], w=[k2])
                S.op('act', lambda e: e.activation(out=e2[:], in_=e2[:], func=AF.Exp, scale=-0.5), r=[k2], w=[k2])
                S.op('dve', lambda e: e.scalar_tensor_tensor(
                    out=catA[:, h, qb * 512:(qb + 1) * 512], in0=e1[:], scalar=gsc[:, 0:1], in1=e2[:], op0=ALU.mult, op1=ALU.mult),
                     r=[k1, k2, 'gsc'], w=[('catA', h)])

            epsb = sb("epsb", [128, 1], F32, esB)
            S.op('dve', lambda e: e.memset(epsb[:], RMS_EPS), w=['epsb'])
            NS = len(steps)
            pending = []
            for n in range(NS + LOOK + 6):
                if n < NS:
                    emit_scores(n)
                while pending and pending[0][0] <= n - LOOK:
                    due, h_, qb_, par_ = pending.pop(0)
                    epilogue2(h_, qb_, par_, n)
                if LOOK <= n < NS + LOOK:
                    emit_pv(n - LOOK)
            S.barrier()

        if stage == 'B':
            tmp = sb("dbgt", [128, T], F32)
            for h in range(4):
                S.op('dve', lambda e, h=h: e.tensor_copy(out=tmp[:], in_=catA[:, h, :]), r=[('catA', h)], w=['dbgt'])
                S.dma('sp', lambda e, h=h: e.dma_start(out=dbg.rearrange("(a b) d -> a (b d)", b=4)[h * 128:(h + 1) * 128, :],
                                                       in_=tmp[:]), 'dbg', r=['dbgt'], w=[('dbg', h)])
            S.finish([('dbg', h) for h in range(4)])
            return nc
        L = dict(nc=nc, S=S, sb=sb, ps=ps, PK=PK, dr=dr, uT=uT, catA=catA, catS=uT, ident=ident, identb=identb, onesf=onesf, onesb=onesb,
                 stage=stage, y=y, dbg=dbg, xscr=xscr, yscr=yscr, hscr=hscr, es=es)
        if not SKIP:
            phase_c(L)
        if stage == 'C0':
            return nc
        if stage == 'C':
            tmp = sb("dbgt", [128, T], F32)
            for h in range(4):
                S.op('dve', lambda e, h=h: e.tensor_copy(out=tmp[:], in_=uT[:, h, :]), r=[('uT', h)], w=['dbgt'])
                S.dma('sp', lambda e, h=h: e.dma_start(out=dbg.rearrange("(a b) d -> a (b d)", b=4)[h * 128:(h + 1) * 128, :],
                                                       in_=tmp[:]), 'dbg', r=['dbgt'], w=[('dbg', h)])
            S.finish([('dbg', h) for h in range(4)])
            return nc
        if os.environ.get('MK_DENSE', '0') == '1':
            phase_de(L)
        else:
            phase_sparse(L)
    return nc


def phase_c(L):
    nc = L['nc']; S = L['S']; sb = L['sb']; ps = L['ps']; dr = L['dr']
    PK = lambda i: ('psC', i)
    uT = L['uT']; ident = L['ident']; identb = L['identb']; stage = L['stage']
    esC = contextlib.ExitStack()
    TWO_PI = 2.0 * math.pi
    with esC:
        def t(name, shape, dt=F32):
            return sb(name, shape, dt, esC)
        def TT(eng, out, a, b, op, r, w):
            S.op(eng, lambda e: e.tensor_tensor(out=out, in0=a, in1=b, op=op), r=r, w=w)
        def TS(eng, out, a, s1, op0, r, w, s2=None, op1=None):
            if op1 is None:
                S.op(eng, lambda e: e.tensor_scalar(out=out, in0=a, scalar1=s1, scalar2=None, op0=op0), r=r, w=w)
            else:
                S.op(eng, lambda e: e.tensor_scalar(out=out, in0=a, scalar1=s1, scalar2=s2, op0=op0, op1=op1), r=r, w=w)
        def STT(out, a, sc, b, op0, op1, r, w):
            S.op('dve', lambda e: e.scalar_tensor_tensor(out=out, in0=a, scalar=sc, in1=b, op0=op0, op1=op1), r=r, w=w)
        def ACT(out, a, func, r, w, scale=1.0, bias=None):
            if bias is None:
                S.op('act', lambda e: e.activation(out=out, in_=a, func=func, scale=scale), r=r, w=w)
            else:
                S.op('act', lambda e: e.activation(out=out, in_=a, func=func, scale=scale, bias=bias), r=r, w=w)
        def LD(dst, src, key, sem='cld'):
            S.dma('sp', lambda e: e.dma_start(out=dst, in_=src), sem, w=[key])

        dcol = t("dcol", [128, 4]); bglu = t("bglu", [128, 4]); rmk = t("rmk", [128, 4])
        wglu = t("wglu", [128, 4, 512], BF16)
        KS = t("KS", [128, 9, 3, 16])
        WCb = t("WCb", [128, 9, 2, 16, 2, 16], BF16)
        Kblk = t("Kblk", [128, 4, 8, 128], BF16)
        W1c = t("W1c", [128, 4, 8, 2, 128], BF16)
        esS = contextlib.ExitStack()
        def ts(name, shape, dt=F32):
            return sb(name, shape, dt, esS)
        are = ts("are", [128, 16]); aim = ts("aim", [128, 16]); ldt = ts("ldt", [128, 16])
        bre = ts("bre", [128, 16, 16]); bim = ts("bim", [128, 16, 16])
        cre = ts("cre", [128, 16, 16]); cim = ts("cim", [128, 16, 16])
        bdm = ts("bdm", [128, 128])
        LD(are[:], dr['s_are'][:, :], 'are', 'c0'); LD(aim[:], dr['s_aim'][:, :], 'aim', 'c1'); LD(ldt[:], dr['s_ldt'][:, :], 'ldt', 'c2')
        LD(bre[:].rearrange("p a b -> p (a b)"), dr['s_bre'][:, :], 'bre', 'c3'); LD(bim[:].rearrange("p a b -> p (a b)"), dr['s_bim'][:, :], 'bim', 'c4')
        LD(cre[:].rearrange("p a b -> p (a b)"), dr['s_cre'][:, :], 'cre', 'c5'); LD(cim[:].rearrange("p a b -> p (a b)"), dr['s_cim'][:, :], 'cim', 'c6')
        LD(dcol[:], dr['s_d'][:, :], 'dcol', 'c7'); LD(bglu[:], dr['s_bglu'][:, :], 'bglu', 'c8')
        LD(bdm[:], dr['c_bdmask'][:, :], 'bdm', 'c9'); LD(rmk[:], dr['c_rmask'][:, :], 'rmk', 'c10')
        for k in range(4):
            S.dma('pool', lambda e, k=k: e.dma_start(out=wglu[:, k, :], in_=dr['w_glu'][k * 128:(k + 1) * 128, :]), 'wglu', w=[('wglu', kk) for kk in range(4)], join=True)

        sm = ts("sm", [128, 14, 16])
        LAM = ts("LAM", [128, 9, 2, 16])
        dtt, xr, xi, mag, fr, msk, cs, sn = [sm[:, i, :] for i in range(8)]
        ACT(dtt, ldt[:], AF.Exp, ['ldt'], ['sm'])
        TT('dve', xr, are[:], dtt, ALU.mult, ['are', 'sm'], ['sm'])
        TT('dve', xi, aim[:], dtt, ALU.mult, ['aim', 'sm'], ['sm'])
        ACT(mag, xr, AF.Exp, ['sm'], ['sm'])
        icv = ts("icv", [128, 16], I32)
        def sin_of(dst, shift):
            TS('dve', fr, xi, 1.0 / TWO_PI, ALU.mult, ['sm'], ['sm'], shift, ALU.add)
            S.op('dve', lambda e: e.tensor_copy(out=icv[:], in_=fr), r=['sm'], w=['icv'])
            S.op('dve', lambda e: e.tensor_copy(out=msk, in_=icv[:]), r=['icv'], w=['sm'])
            TT('dve', fr, fr, msk, ALU.subtract, ['sm'], ['sm'])
            TS('dve', msk, fr, 0.5, ALU.is_gt, ['sm'], ['sm'])
            TT('dve', fr, fr, msk, ALU.subtract, ['sm'], ['sm'])
            TS('dve', msk, fr, -0.5, ALU.is_lt, ['sm'], ['sm'])
            TT('dve', fr, fr, msk, ALU.add, ['sm'], ['sm'])
            ACT(dst, fr, AF.Sin, ['sm'], ['sm'], scale=TWO_PI)
        sin_of(sn, 0.0)
        sin_of(cs, 0.25)
        S.op('dve', lambda e: e.memset(LAM[:, 0, 0, :], 1.0), w=['LAM'])
        S.op('dve', lambda e: e.memset(LAM[:, 0, 1, :], 0.0), w=['LAM'])
        TT('dve', LAM[:, 1, 0, :], mag, cs, ALU.mult, ['sm'], ['LAM'])
        TT('dve', LAM[:, 1, 1, :], mag, sn, ALU.mult, ['sm'], ['LAM'])
        t0, t1, t2, t3 = sm[:, 8, :], sm[:, 9, :], sm[:, 12, :], sm[:, 13, :]
        def cmul(or_, oi_, ar_, ai_, br_, bi_, keys_r, keys_w):
            TT('dve', t0, ar_, br_, ALU.mult, keys_r, ['smt0'])
            TT('dve', t1, ai_, bi_, ALU.mult, keys_r, ['smt1'])
            TT('dve', t2, ar_, bi_, ALU.mult, keys_r, ['smt2'])
            TT('dve', t3, ai_, br_, ALU.mult, keys_r, ['smt3'])
            TT('dve', or_, t0, t1, ALU.subtract, ['smt0', 'smt1'], keys_w)
            TT('dve', oi_, t2, t3, ALU.add, ['smt2', 'smt3'], keys_w)
        for n in range(2, 9):
            cmul(LAM[:, n, 0, :], LAM[:, n, 1, :], LAM[:, n - 1, 0, :], LAM[:, n - 1, 1, :], LAM[:, 1, 0, :], LAM[:, 1, 1, :], ['LAM'], ['LAM'])
        S.op('dve', lambda e: e.tensor_copy(out=KS[:, 0, 0:2, :], in_=LAM[:, 8, :, :]), r=['LAM'], w=['KS'])
        for k in range(1, 9):
            cmul(KS[:, k, 0, :], KS[:, k, 1, :], KS[:, k - 1, 0, :], KS[:, k - 1, 1, :], KS[:, k - 1, 0, :], KS[:, k - 1, 1, :], ['KS'], ['KS'])
        TS('dve', KS[:, :, 2, :], KS[:, :, 1, :], -1.0, ALU.mult, ['KS'], ['KS'])
        cfr, cfi = sm[:, 10, :], sm[:, 11, :]
        lm1 = sm[:, 4, :]; den = sm[:, 5, :]
        TS('dve', lm1, LAM[:, 1, 0, :], -1.0, ALU.add, ['LAM'], ['sm'])
        TT('dve', t0, are[:], are[:], ALU.mult, ['are'], ['sm'])
        TT('dve', t1, aim[:], aim[:], ALU.mult, ['aim'], ['sm'])
        TT('dve', den, t0, t1, ALU.add, ['sm'], ['sm'])
        S.op('dve', lambda e: e.reciprocal(out=den, in_=den), r=['sm'], w=['sm'])
        TT('dve', t0, lm1, are[:], ALU.mult, ['sm', 'are'], ['sm'])
        TT('dve', t1, LAM[:, 1, 1, :], aim[:], ALU.mult, ['LAM', 'aim'], ['sm'])
        TT('dve', cfr, t0, t1, ALU.add, ['sm'], ['sm'])
        TT('dve', cfr, cfr, den, ALU.mult, ['sm'], ['sm'])
        TT('dve', t0, LAM[:, 1, 1, :], are[:], ALU.mult, ['LAM', 'are'], ['sm'])
        TT('dve', t1, lm1, aim[:], ALU.mult, ['sm', 'aim'], ['sm'])
        TT('dve', cfi, t0, t1, ALU.subtract, ['sm'], ['sm'])
        TT('dve', cfi, cfi, den, ALU.mult, ['sm'], ['sm'])
        big = ts("big", [128, 8, 16, 16])
        bbr, bbi, w0, w1, w2, w3, w4, w5 = [big[:, i, :, :] for i in range(8)]
        def bc(v):
            return v.unsqueeze(2).to_broadcast([128, 16, 16])
        def cmul_b(or_, oi_, vr, vi, mr, mi, kr, kw):
            TT('dve', w0, mr, bc(vr), ALU.mult, kr, ['bw0'])
            TT('dve', w1, mi, bc(vi), ALU.mult, kr, ['bw1'])
            TT('dve', w4, mi, bc(vr), ALU.mult, kr, ['bw4'])
            TT('dve', w5, mr, bc(vi), ALU.mult, kr, ['bw5'])
            TT('dve', or_, w0, w1, ALU.subtract, ['bw0', 'bw1'], kw)
            TT('dve', oi_, w4, w5, ALU.add, ['bw4', 'bw5'], kw)
        cmul_b(bbr, bbi, cfr, cfi, bre[:], bim[:], ['sm', 'bre', 'bim'], ['big'])
        XMb = ts("XMb", [128, 8, 2, 16, 2, 16], BF16)
        S.op('pool', lambda e: e.memset(XMb[:].rearrange("p a b c d e -> p (a b c d e)"), 0.0), w=['XMb'])
        S.op('pool', lambda e: e.memset(WCb[:].rearrange("p a b c d e -> p (a b c d e)"), 0.0), w=['WCb'])
        for n in range(8):
            cmul_b(w2, w3, LAM[:, n, 0, :], LAM[:, n, 1, :], bbr, bbi, ['LAM', 'big'], ['big'])
            for g2 in range(2):
                lo = 64 * g2
                S.op('act', lambda e, n=n, g2=g2, lo=lo: e.activation(out=XMb[lo:lo + 64, n, 0, :, g2, :], in_=big[lo:lo + 64, 4, :, :], func=AF.Copy), r=['big'], w=['XMb'])
                S.op('act', lambda e, n=n, g2=g2, lo=lo: e.activation(out=XMb[lo:lo + 64, n, 1, :, g2, :], in_=big[lo:lo + 64, 5, :, :], func=AF.Copy), r=['big'], w=['XMb'])
        for n in range(9):
            cmul_b(w2, w3, LAM[:, n, 0, :], LAM[:, n, 1, :], cre[:], cim[:], ['LAM', 'cre', 'cim'], ['big'])
            for g2 in range(2):
                lo = 64 * g2
                S.op('act', lambda e, n=n, g2=g2, lo=lo: e.activation(out=WCb[lo:lo + 64, n, 0, :, g2, :], in_=big[lo:lo + 64, 4, :, :], func=AF.Copy), r=['big'], w=['WCb'])
                S.op('act', lambda e, n=n, g2=g2, lo=lo: e.activation(out=WCb[lo:lo + 64, n, 1, :, g2, :], in_=big[lo:lo + 64, 5, :, :], func=AF.Copy, scale=-1.0), r=['big'], w=['WCb'])
        kf = ts("kf", [128, 128])
        pi = 0
        for b in range(4):
            for tau in range(8):
                pb = pi % 8; pi += 1
                for qp in range(4):
                    for ri in range(2):
                        S.op('pe', lambda e, pb=pb, b=b, tau=tau, qp=qp, ri=ri: e.matmul(
                            ps[pb][:, qp * 32:(qp + 1) * 32],
                            lhsT=XMb[:, tau, ri, 4 * b:4 * b + 4, :, :].rearrange("p a b c -> p (a b c)"),
                            rhs=WCb[:, 0, ri, 4 * b + qp, :, :].rearrange("p a b -> p (a b)"),
                            start=(ri == 0), stop=(ri == 1)), r=['XMb', 'WCb'], w=[PK(pb)])
                if tau == 0:
                    TT('dve', kf[:], ps[pb][:, 0:128], bdm[:], ALU.mult, [PK(pb), 'bdm'], ['kf'])
                    STT(Kblk[:, b, 0, :], ident[:], dcol[:, b:b + 1], kf[:], ALU.mult, ALU.add, ['kf', 'ident', 'dcol'], ['Kblk'])
                else:
                    TT('dve', Kblk[:, b, tau, :], ps[pb][:, 0:128], bdm[:], ALU.mult, [PK(pb), 'bdm'], ['Kblk'])
            for s_ in range(8):
                for ri in range(2):
                    pb = pi % 8; pi += 1
                    pbf = ps[pb][:, :].bitcast(BF16)
                    S.op('pe', lambda e, pbf=pbf, b=b, s_=s_, ri=ri: e.transpose(
                        out=pbf[:, 0:128], in_=XMb[:, 7 - s_, ri, 4 * b:4 * b + 4, :, :].rearrange("p a b c -> p (a b c)"), identity=identb[:]),
                         r=['XMb', 'identb'], w=[PK(pb)])
                    S.op('act', lambda e, pbf=pbf, b=b, s_=s_, ri=ri: e.activation(out=W1c[:, b, s_, ri, :], in_=pbf[:, 0:128], func=AF.Copy),
                         r=[PK(pb)], w=['W1c'])

        S.barrier()
        esS.close()
        if stage == 'C0':
            return
        catS = L['catS']
        zblk = t("zblk", [128, T], BF16)
        A0 = [t(f"A0{i}", [128, 4, 2, NCH]) for i in range(2)]; tmpk = t("tmpk", [128, 2, NCH])
        Hb = [t(f"Hb{i}", [128, 4, 2, NCH], BF16) for i in range(2)]
        WCm = t("WCm", [128, 8, 2, 4, 128], BF16)
        W1m = [t(f"W1m{i}", [128, 8, 2, 128], BF16) for i in range(2)]
        yb_ = [t(f"ybuf{i}", [128, 512]) for i in range(2)]
        gl = [t(f"gl{i}", [128, 512]) for i in range(3)]
        S.op('pool', lambda e: e.memset(WCm[:].rearrange("p a b c d -> p (a b c d)"), 0.0), w=['WCm'])
        for i in range(2):
            S.op('pool', lambda e, i=i: e.memset(Hb[i][:, :, :, 0:1].rearrange("p a b c -> p (a b c)"), 0.0), w=[('Hb', i)])

        def statein(b):
            nonlocal pi
            uk = ('uT', b); par = b % 2
            for qp in range(4):
                wm = W1m[qp % 2]; wmk = ('W1m', qp % 2)
                S.op('act', lambda e, wm=wm, qp=qp: e.activation(out=wm[:].rearrange("p a b c -> p (a b c)"),
                                                              in_=W1c[:, b, :, :, :].rearrange("p a b c -> p (a b c)"), func=AF.Copy, scale=rmk[:, qp:qp + 1]),
                     r=['W1c', 'rmk'], w=[wmk])
                for ri in range(2):
                    pb = pi % 8; pi += 1
                    for s_ in range(8):
                        S.op('pe', lambda e, pb=pb, s_=s_, ri=ri, wm=wm: e.matmul(
                            ps[pb][:, :], lhsT=wm[:, s_, ri, :], rhs=uT[:, b, s_ * NCH:(s_ + 1) * NCH], start=(s_ == 0), stop=(s_ == 7)),
                             r=[wmk, uk], w=[PK(pb)])
                    S.op('act', lambda e, pb=pb, qp=qp, ri=ri: e.activation(out=A0[par][:, qp, ri, :], in_=ps[pb][:, :], func=AF.Copy),
                         r=[PK(pb)], w=[('A0', par, qp)])

        def kscan(b):
            par = b % 2
            for qp in range(4):
                q = 4 * b + qp
                ak = ('A0', par, qp)
                cur = A0[par][:, qp, :, :]; nxt = tmpk[:, :, :]
                ck, nk = ak, 'tmpk'
                for k in range(9):
                    d_ = 1 << k
                    ar_ = KS[:, k, 0, q:q + 1]; ai_ = KS[:, k, 1, q:q + 1]; nai_ = KS[:, k, 2, q:q + 1]
                    nk0 = (nk, 0); nk1 = (nk, 1)
                    STT(nxt[:, 0, d_:], cur[:, 0, 0:NCH - d_], ar_, cur[:, 0, d_:], ALU.mult, ALU.add, [ck, 'KS'], [nk, nk0])
                    STT(nxt[:, 1, d_:], cur[:, 1, 0:NCH - d_], ar_, cur[:, 1, d_:], ALU.mult, ALU.add, [ck, 'KS'], [nk1])
                    STT(nxt[:, 0, d_:], cur[:, 1, 0:NCH - d_], nai_, nxt[:, 0, d_:], ALU.mult, ALU.add, [ck, nk0, 'KS'], [nk0])
                    STT(nxt[:, 1, d_:], cur[:, 0, 0:NCH - d_], ai_, nxt[:, 1, d_:], ALU.mult, ALU.add, [ck, nk1, 'KS'], [nk1, nk])
                    S.op('dve', lambda e, cur=cur, nxt=nxt, d_=d_: e.tensor_copy(out=nxt[:, :, 0:d_], in_=cur[:, :, 0:d_]), r=[ck], w=[nk])
                    cur, nxt = nxt, cur; ck, nk = nk, ck
                S.op('dve', lambda e, cur=cur, qp=qp: e.tensor_copy(out=Hb[par][:, qp, :, 1:NCH], in_=cur[:, :, 0:NCH - 1]),
                     r=[ck], w=[('Hb', par)])

        def outputs(b):
            nonlocal pi
            uk = ('uT', b); par = b % 2
            for qp in range(4):
                S.op('act', lambda e, qp=qp: e.activation(out=WCm[:, :, :, qp, 32 * qp:32 * qp + 32],
                                                          in_=WCb[:, 1:9, :, 4 * b + qp, :, :].rearrange("p a b c d -> p a b (c d)"), func=AF.Copy),
                     r=['WCb'], w=['WCm'])
            for s_ in range(8):
                pb = pi % 8; pi += 1
                for sp_ in range(s_ + 1):
                    S.op('pe', lambda e, pb=pb, s_=s_, sp_=sp_: e.matmul(
                        ps[pb][:, :], lhsT=Kblk[:, b, s_ - sp_, :], rhs=uT[:, b, sp_ * NCH:(sp_ + 1) * NCH], start=(sp_ == 0), stop=False),
                         r=['Kblk', uk], w=[PK(pb)])
                for qp in range(4):
                    for ri in range(2):
                        S.op('pe', lambda e, pb=pb, s_=s_, qp=qp, ri=ri: e.matmul(
                            ps[pb][:, :], lhsT=WCm[:, s_, ri, qp, :], rhs=Hb[par][:, qp, ri, :], start=False, stop=(qp == 3 and ri == 1)),
                             r=['WCm', ('Hb', par)], w=[PK(pb)])
                yy = yb_[s_ % 2]; yk = ('ybuf', s_ % 2)
                g0, g1, g2_ = gl
                ACT(yy[:], ps[pb][:, :], AF.Copy, [PK(pb)], [yk])
                ACT(g0[:], ps[pb][:, :], AF.Square, [PK(pb)], ['g0'], scale=0.044715 ** 0.5)
                TT('pool', g1[:], g0[:], yy[:], ALU.mult, ['g0', yk], ['g1'])
                TT('pool', g1[:], g1[:], yy[:], ALU.add, ['g1', yk], ['g1'])
                ACT(g2_[:], g1[:], AF.Sigmoid, ['g1'], ['g2'], scale=1.5957691216057308)
                TT('pool', zblk[:, s_ * NCH:(s_ + 1) * NCH], g2_[:], yy[:], ALU.mult, ['g2', yk], ['zblk'])
            S.op('act', lambda e: e.activation(out=uT[:, b, :].rearrange("p (c s) -> p s c", s=LCH), in_=zblk[:].rearrange("p (s c) -> p s c", s=LCH), func=AF.Copy), r=['zblk', uk], w=[uk])

        statein(0)
        kscan(0)
        for b in range(4):
            if b + 1 < 4:
                statein(b + 1)
                kscan(b + 1)
            outputs(b)
        gg = [gl[0], gl[1], gl[2], yb_[0]]; ggk = ['g0', 'g1', 'g2', ('ybuf', 0)]
        for tb in range(8):
            pbs = []
            for fo in range(4):
                pb = pi % 8; pi += 1
                pbs.append(pb)
                for k in range(4):
                    S.op('pe', lambda e, pb=pb, k=k, fo=fo: e.matmul(
                        ps[pb][:, :], lhsT=wglu[:, k, fo * 128:(fo + 1) * 128], rhs=uT[:, k, tb * 512:(tb + 1) * 512],
                        start=(k == 0), stop=(k == 3)), r=[('wglu', k), ('uT', k), ('uTg', k, tb)], w=[PK(pb)])
            for fo in range(4):
                ACT(gg[fo][:], ps[pbs[fo]][:, :], AF.Sigmoid, [PK(pbs[fo]), 'bglu'], [ggk[fo]], bias=bglu[:, fo:fo + 1])
            for fo in range(4):
                eng = 'dve' if fo % 2 == 0 else 'pool'
                TT(eng, uT[:, fo, tb * 512:(tb + 1) * 512], gg[fo][:], uT[:, fo, tb * 512:(tb + 1) * 512], ALU.mult,
                   [ggk[fo], ('uT', fo)], [('uTg', fo, tb)])
        S.barrier()


def phase_de(L):
    nc = L['nc']; S = L['S']; sb = L['sb']; ps = L['ps']; PK = L['PK']; dr = L['dr']
    uT = L['uT']; catA = L['catA']; catS = L['catS']; ident = L['ident']; y = L['y']; stage = L['stage']; dbg = L['dbg']
    esD = contextlib.ExitStack()
    with esD:
        def t(name, shape, dt=F32):
            return sb(name, shape, dt, esD)
        def TT(eng, out, a, b, op, r, w):
            S.op(eng, lambda e: e.tensor_tensor(out=out, in0=a, in1=b, op=op), r=r, w=w)
        def TS(eng, out, a, s1, op0, r, w, s2=None, op1=None):
            if op1 is None:
                S.op(eng, lambda e: e.tensor_scalar(out=out, in0=a, scalar1=s1, scalar2=None, op0=op0), r=r, w=w)
            else:
                S.op(eng, lambda e: e.tensor_scalar(out=out, in0=a, scalar1=s1, scalar2=s2, op0=op0, op1=op1), r=r, w=w)
        def STT(out, a, sc, b, op0, op1, r, w):
            S.op('dve', lambda e: e.scalar_tensor_tensor(out=out, in0=a, scalar=sc, in1=b, op0=op0, op1=op1), r=r, w=w)
        def ACT(out, a, func, r, w, scale=1.0, bias=None):
            if bias is None:
                S.op('act', lambda e: e.activation(out=out, in_=a, func=func, scale=scale), r=r, w=w)
            else:
                S.op('act', lambda e: e.activation(out=out, in_=a, func=func, scale=scale, bias=bias), r=r, w=w)

        def catv(k):
            return (catA[:, k, :], ('catA', k)) if k < 4 else (uT[:, k - 4, :], ('uT', k - 4))

        woutb = t("woutb", [128, 8, D], BF16)
        for k in range(8):
            S.dma('pool', lambda e, k=k: e.dma_start(out=woutb[:, k, :], in_=dr['w_out'][k * 128:(k + 1) * 128, :]), 'wout', w=[('woutb', kk) for kk in range(8)], join=True)
        wrh = t("wrh", [128, 8, 36], BF16); wrl = t("wrl", [128, 8, 36], BF16)
        hTlo = t("hTlo", [128, 8, 128], BF16)
        lnp = t("lnp", [128, 2, D])
        HT = 16
        acc = t("acc", [128, HT, D])
        Wt = t("Wt", [128, HT, 32])
        xs = [t("xsd0", [128, D])]
        wr = xs[0][:, 0:288].rearrange("p (a b) -> p a b", a=8)
        S.dma('sp', lambda e: e.dma_start(out=wr, in_=dr['w_r'].rearrange("(k p) f -> p k f", p=128)), 'c0', w=['hTf'])
        S.op('dve', lambda e: e.tensor_copy(out=wrh[:], in_=wr), r=['hTf'], w=['wrh'])
        S.op('dve', lambda e: e.tensor_tensor(out=wrl[:], in0=wr, in1=wrh[:], op=ALU.subtract), r=['hTf', 'wrh'], w=['wrl'])
        st = t("st", [128, 2, 6]); mv = t("mv", [128, 4]); rt = t("rt", [128, 64]); rs = t("rs", [128, 32])
        Wg = [t(f"Wg{i}", [128, 8, 512], BF16) for i in range(2)]
        Wu = [t(f"Wu{i}", [128, 8, 512], BF16) for i in range(2)]
        Wd = [t("Wd0", [128, 4, D], BF16)]
        hidT = [t("hidT0", [128, 4, 512], BF16)]
        pi = 0

        def layernorm(src, dst, gi, keys_r, keys_w):
            S.op('dve', lambda e: e.bn_stats(out=st[:, 0, :], in_=src[:, 0:512]), r=keys_r, w=['st'])
            S.op('dve', lambda e: e.bn_stats(out=st[:, 1, :], in_=src[:, 512:1024]), r=keys_r, w=['st'])
            S.op('dve', lambda e: e.bn_aggr(out=mv[:, 0:2], in_=st[:].rearrange("p a b -> p (a b)")), r=['st'], w=['mv'])
            ACT(mv[:, 2:3], mv[:, 1:2], AF.Sqrt, ['mv'], ['mv'], bias=LN_EPS)
            S.op('dve', lambda e: e.reciprocal(out=mv[:, 3:4], in_=mv[:, 2:3]), r=['mv'], w=['mv'])
            TS('dve', dst, src, mv[:, 0:1], ALU.subtract, keys_r + ['mv'], keys_w, mv[:, 3:4], ALU.mult)
            TT('dve', dst, dst, lnp[:, gi, :], ALU.mult, keys_w + ['lnp'], keys_w)
            TT('pool', dst, dst, lnp[:, gi + 1, :], ALU.add, keys_w + ['lnp'], keys_w)

        xq = 0
        CUT = int(os.environ.get('MK_CUT', '0'))
        def cut(n, keys):
            if CUT == n:
                S.barrier()
                return True
            return False
        for hf in range(1 if stage in ('D', 'E') else 2):
            S.dma('sp', lambda e: e.dma_start(out=lnp[:, 0:2, :].rearrange("p a b -> p (a b)"),
                                              in_=dr['ln1'].rearrange("a b -> (a b)").partition_broadcast(128)), 'c1', w=['lnp'])
            for il in range(2 if stage == 'D' else HT):
                ti = hf * HT + il
                tok = slice(ti * 128, (ti + 1) * 128)
                ak = ('acc', il)
                xsi = xs[0]; xsk = 'hTf'; xsn = 'xsd0'; xq += 1
                S.dma('sp', lambda e, xsi=xsi, ti=ti: e.dma_start(out=xsi[:], in_=dr['x'][ti * 128:(ti + 1) * 128, :]), xsn, w=[xsk])
                for half in range(2):
                    pb = pi % 8; pi += 1
                    for k in range(8):
                        cv, ck = catv(k)
                        S.op('pe', lambda e, pb=pb, k=k, half=half, cv=cv, tok=tok: e.matmul(
                            ps[pb][:, :], lhsT=cv[:, tok], rhs=woutb[:, k, half * 512:(half + 1) * 512], start=(k == 0), stop=(k == 7)),
                             r=[ck, ('woutb', k)], w=[PK(pb)])
                    STT(acc[:, il, half * 512:(half + 1) * 512], xsi[:, half * 512:(half + 1) * 512], ALPHA, ps[pb][:, :], ALU.mult, ALU.add,
                        [xsk, PK(pb)], [ak])
                if cut(1, [ak]): return
                layernorm(acc[:, il, :], acc[:, il, :], 0, [ak], [ak])
                if cut(2, [ak]): return
                for half in range(2):
                    pb = pi % 8; pi += 1
                    for kk in range(4):
                        k = half * 4 + kk
                        S.op('pe', lambda e, pb=pb, kk=kk, k=k, il=il: e.transpose(
                            out=ps[pb][:, kk * 128:(kk + 1) * 128], in_=acc[:, il, k * 128:(k + 1) * 128], identity=ident[:]),
                             r=[ak, 'ident'], w=[PK(pb)])
                    for kk in range(4):
                        k = half * 4 + kk
                        cv, ck = catv(k)
                        S.op('act', lambda e, pb=pb, kk=kk, cv=cv, tok=tok: e.activation(out=cv[:, tok], in_=ps[pb][:, kk * 128:(kk + 1) * 128], func=AF.Copy),
                             r=[PK(pb)], w=[ck])
                if cut(6, [ak]): return
                pb = pi % 8; pi += 1
                for k in range(8):
                    cv, ck = catv(k)
                    S.op('pe', lambda e, pb=pb, k=k, cv=cv, tok=tok: e.matmul(ps[pb][:, 0:36], lhsT=cv[:, tok], rhs=wrh[:, k, :], start=(k == 0), stop=(k == 7)),
                         r=[ck, 'wrh'], w=[PK(pb)])
                if cut(3, [ak]): return
                R = ['rt']
                lg = rt[:, 0:36]
                S.op('dve', lambda e, pb=pb: e.tensor_copy(out=rt[:, 0:36], in_=ps[pb][:, 0:36]), r=[PK(pb)], w=R)
                mx = rt[:, 36:37]; nmx = rt[:, 37:38]; ohg = rt[:, 38:42]; ex = rt[:, 42:46]; gp = rt[:, 46:47]
                sel = rt[:, 47:55]; m1 = rt[:, 55:56]; oh1 = rt[:, 56:64]
                S.op('dve', lambda e: e.reduce_max(out=mx, in_=rt[:, 0:4], axis=AX.X), r=R, w=R)
                TS('dve', ohg, rt[:, 0:4], mx, ALU.is_equal, R, R)
                TS('dve', nmx, mx, -1.0, ALU.mult, R, R)
                ACT(ex, rt[:, 0:4], AF.Exp, R, R, bias=nmx)
                S.op('dve', lambda e: e.reduce_sum(out=gp, in_=ex, axis=AX.X), r=R, w=R)
                S.op('dve', lambda e: e.reciprocal(out=gp, in_=gp), r=R, w=R)
                TS('dve', sel, rt[:, 4:12], ohg[:, 0:1], ALU.mult, R, R)
                for g in range(1, 4):
                    STT(sel, rt[:, 4 + 8 * g:12 + 8 * g], ohg[:, g:g + 1], sel, ALU.mult, ALU.add, R, R)
                S.op('dve', lambda e: e.reduce_max(out=m1, in_=sel, axis=AX.X), r=R, w=R)
                TS('dve', oh1, sel, m1, ALU.is_equal, R, R)
                sel2 = rs[:, 0:8]; m2 = rs[:, 8:9]; oh2 = rs[:, 9:17]; pa = rs[:, 17:18]; wa = rs[:, 18:19]; wb = rs[:, 19:20]; w8 = rs[:, 20:28]
                RS = ['rs']
                STT(sel2, oh1, -1e30, sel, ALU.mult, ALU.add, R, RS)
                S.op('dve', lambda e: e.reduce_max(out=m2, in_=sel2, axis=AX.X), r=RS, w=RS)
                TS('dve', oh2, sel2, m2, ALU.is_equal, RS, RS)
                TT('dve', pa, m1, m2, ALU.subtract, R + RS, RS)
                ACT(pa, pa, AF.Sigmoid, RS, RS)
                TT('dve', wa, pa, gp, ALU.mult, R + RS, RS)
                TT('dve', wb, gp, wa, ALU.subtract, R + RS, RS)
                TS('dve', w8, oh1, wa, ALU.mult, R + RS, RS)
                STT(w8, oh2, wb, w8, ALU.mult, ALU.add, RS, RS)
                for g in range(4):
                    TS('dve', Wt[:, il, 8 * g:8 * g + 8], w8, ohg[:, g:g + 1], ALU.mult, R + RS, [('Wt', il)])
                if cut(4, [ak]): return
                TS('pool', acc[:, il, :], acc[:, il, :], ALPHA, ALU.mult, [ak], [ak])
            if stage == 'D':
                for il in range(2):
                    S.dma('sp', lambda e, il=il: e.dma_start(out=dbg[il * 128:(il + 1) * 128, :], in_=acc[:, il, :]), f'yo{il}', r=[('acc', il)], w=[('y', il)])
                    S.dma('sp', lambda e, il=il: e.dma_start(out=dbg[1024 + il * 128:1024 + (il + 1) * 128, 0:32], in_=Wt[:, il, :]), f'yo{il + 2}', r=[('Wt', il)], w=[('y', il + 2)])
                S.finish([('y', i) for i in range(4)])
                return
            for ex_ in range(2 if stage == 'E' else NEXP):
                wi = ex_ % 2
                S.dma('pool', lambda e, ex_=ex_, wi=wi: e.dma_start(out=Wg[wi][:], in_=dr['w_exp_gate'][ex_].rearrange("(k p) f -> p k f", p=128)), f'wg{wi}', w=[('Wg', wi)])
                S.dma('pool', lambda e, ex_=ex_, wi=wi: e.dma_start(out=Wu[wi][:], in_=dr['w_exp_up'][ex_].rearrange("(k p) f -> p k f", p=128)), f'wu{wi}', w=[('Wu', wi)])
                S.dma('pool', lambda e, ex_=ex_, wi=wi: e.dma_start(out=Wd[0][:], in_=dr['w_exp_down'][ex_].rearrange("(k p) f -> p k f", p=128)), 'wd0', w=[('Wd', 0)])
                for tb in range(4):
                    t0 = hf * 2048 + tb * 512
                    hid = hidT[0]; hk = ('hidT', 0)
                    for fc in range(4):
                        pg = pi % 8; pi += 1
                        pu = pi % 8; pi += 1
                        for k in range(8):
                            cv, ck = catv(k)
                            S.op('pe', lambda e, pg=pg, k=k, fc=fc, cv=cv, t0=t0, wi=wi: e.matmul(
                                ps[pg][:, :], lhsT=Wg[wi][:, k, fc * 128:(fc + 1) * 128], rhs=cv[:, t0:t0 + 512], start=(k == 0), stop=(k == 7)),
                                 r=[('Wg', wi), ck], w=[PK(pg)])
                        for k in range(8):
                            cv, ck = catv(k)
                            S.op('pe', lambda e, pu=pu, k=k, fc=fc, cv=cv, t0=t0, wi=wi: e.matmul(
                                ps[pu][:, :], lhsT=Wu[wi][:, k, fc * 128:(fc + 1) * 128], rhs=cv[:, t0:t0 + 512], start=(k == 0), stop=(k == 7)),
                                 r=[('Wu', wi), ck], w=[PK(pu)])
                        ACT(hid[:, fc, :], ps[pg][:, :], AF.Silu, [PK(pg)], [hk])
                        TT('dve', hid[:, fc, :], hid[:, fc, :], ps[pu][:, :], ALU.mult, [hk, PK(pu)], [hk])
                    for i4 in range(4):
                        il = tb * 4 + i4
                        for half in range(2):
                            pb = pi % 8; pi += 1
                            for fc in range(4):
                                S.op('pe', lambda e, pb=pb, fc=fc, i4=i4, half=half, hid=hid, wi=wi: e.matmul(
                                    ps[pb][:, :], lhsT=hid[:, fc, i4 * 128:(i4 + 1) * 128], rhs=Wd[0][:, fc, half * 512:(half + 1) * 512],
                                    start=(fc == 0), stop=(fc == 3)), r=[hk, ('Wd', 0)], w=[PK(pb)])
                            STT(acc[:, il, half * 512:(half + 1) * 512], ps[pb][:, :], Wt[:, il, ex_:ex_ + 1], acc[:, il, half * 512:(half + 1) * 512],
                                ALU.mult, ALU.add, [PK(pb), ('Wt', il), ('acc', il)], [('acc', il)])
            S.dma('sp', lambda e: e.dma_start(out=lnp[:, 0:2, :].rearrange("p a b -> p (a b)"),
                                              in_=dr['ln2'].rearrange("a b -> (a b)").partition_broadcast(128)), 'c1', w=['lnp'])
            for il in range(HT):
                ti = hf * HT + il
                layernorm(acc[:, il, :], acc[:, il, :], 0, [('acc', il)], [('acc', il)])
                S.dma('sp', lambda e, il=il, ti=ti: e.dma_start(out=y[ti * 128:(ti + 1) * 128, :], in_=acc[:, il, :]), f'yo{il}',
                      r=[('acc', il)], w=[('y', ti), ('acc', il)])
        S.finish([('y', ti) for ti in range(16 if stage == 'E' else NT)])


def phase_sparse(L):
    nc = L['nc']; S = L['S']; sb = L['sb']; ps = L['ps']; dr = L['dr']
    PK = lambda i: ('psE', i)
    uT = L['uT']; catA = L['catA']; catS = L['catS']; ident = L['ident']; identb = L['identb']; onesb = L['onesb']
    y = L['y']; xscr = L['xscr']; yscr = L['yscr']; hscr = L['hscr']
    esD = contextlib.ExitStack()
    with esD:
        def t(name, shape, dt=F32, st=esD):
            return sb(name, shape, dt, st)
        def TT(eng, out, a, b, op, r, w):
            S.op(eng, lambda e: e.tensor_tensor(out=out, in0=a, in1=b, op=op), r=r, w=w)
        def TS(eng, out, a, s1, op0, r, w, s2=None, op1=None):
            if op1 is None:
                S.op(eng, lambda e: e.tensor_scalar(out=out, in0=a, scalar1=s1, scalar2=None, op0=op0), r=r, w=w)
            else:
                S.op(eng, lambda e: e.tensor_scalar(out=out, in0=a, scalar1=s1, scalar2=s2, op0=op0, op1=op1), r=r, w=w)
        def STT(out, a, sc, b, op0, op1, r, w):
            S.op('dve', lambda e: e.scalar_tensor_tensor(out=out, in0=a, scalar=sc, in1=b, op0=op0, op1=op1), r=r, w=w)
        def ACT(out, a, func, r, w, scale=1.0, bias=None):
            if bias is None:
                S.op('act', lambda e: e.activation(out=out, in_=a, func=func, scale=scale), r=r, w=w)
            else:
                S.op('act', lambda e: e.activation(out=out, in_=a, func=func, scale=scale, bias=bias), r=r, w=w)
        def catv(k):
            return (catA[:, k, :], ('catA', k)) if k < 4 else (uT[:, k - 4, :], ('uT', k - 4))

        slots = t("slots", [128, NT * 2], I32)
        wts = t("wts", [128, NT, 2])
        lnp = t("lnp", [128, 2, D])
        st = t("st", [128, 12, 6]); mv = t("mv", [128, 48]); epsl = t("epsl", [128, 1])
        S.op('dve', lambda e: e.memset(epsl[:], LN_EPS), w=['epsl'])
        Wg = [t(f"Wg{i}", [128, 8, 512], BF16) for i in range(2)]
        Wu = [t(f"Wu{i}", [128, 8, 512], BF16) for i in range(2)]
        Wd = [t(f"Wd{i}", [128, 4, D], BF16) for i in range(2)]

        def load_expert(ex_):
            wi = ex_ % 2
            S.dma('pool', lambda e: e.dma_start(out=Wg[wi][:], in_=dr['w_exp_gate'][ex_].rearrange("(k p) f -> p k f", p=128)), f'wg{wi}', w=[('Wg', wi)])
            S.dma('pool', lambda e: e.dma_start(out=Wu[wi][:], in_=dr['w_exp_up'][ex_].rearrange("(k p) f -> p k f", p=128)), f'wu{wi}', w=[('Wu', wi)])
            S.dma('pool', lambda e: e.dma_start(out=Wd[wi][:], in_=dr['w_exp_down'][ex_].rearrange("(k p) f -> p k f", p=128)), f'wd{wi}', w=[('Wd', wi)])

        def ln_stats(src, keys_r, slot, epst=None, part=0):
            epst = epsl if epst is None else epst
            st_ = st[:, 2 * slot:2 * slot + 2, :]; mv_ = mv[:, 8 * slot:8 * slot + 8]; mk = ('mv', slot)
            S.op('dve', lambda e: e.bn_stats(out=st_[:, 0, :], in_=src[:, 0:512]), r=keys_r, w=[mk])
            S.op('dve', lambda e: e.bn_stats(out=st_[:, 1, :], in_=src[:, 512:1024]), r=keys_r, w=[mk])
            S.op('dve', lambda e: e.bn_aggr(out=mv_[:, 0:2], in_=st_.rearrange("p a b -> p (a b)")), r=[mk], w=[mk])
            if part == 1:
                return
            ln_stats_b(slot, epst)

        def ln_stats_b(slot, epst):
            mv_ = mv[:, 8 * slot:8 * slot + 8]; mk = ('mv', slot)
            ACT(mv_[:, 2:3], mv_[:, 1:2], AF.Ln, [mk, 'epsl'], [mk], bias=epst[:, 0:1])
            ACT(mv_[:, 3:4], mv_[:, 2:3], AF.Exp, [mk], [mk], scale=-0.5)
            STT(mv_[:, 4:5], mv_[:, 0:1], -1.0, mv_[:, 3:4], ALU.mult, ALU.mult, [mk], [mk])

        def ln_apply(src, dst, keys_r, keys_w, slot, beng='dve', part=None):
            mv_ = mv[:, 8 * slot:8 * slot + 8]; mk = ('mv', slot)
            if part in (None, 'a'):
                S.op('act', lambda e: e.activation(out=dst, in_=src, func=AF.Identity, scale=mv_[:, 3:4], bias=mv_[:, 4:5]), r=keys_r + [mk], w=keys_w)
            if part in (None, 'b'):
                TT('dve', dst, dst, lnp[:, 0, :], ALU.mult, keys_w + ['lnp'], keys_w)
                TT(beng, dst, dst, lnp[:, 1, :], ALU.add, keys_w + ['lnp'], keys_w)

        pi = 0
        esd = contextlib.ExitStack()
        woutb = t("woutb", [128, 8, D], BF16, esd)
        for k in range(8):
            S.dma('pool', lambda e, k=k: e.dma_start(out=woutb[:, k, :], in_=dr['w_out'][k * 128:(k + 1) * 128, :]), 'wout', w=[('woutb', kk) for kk in range(8)], join=True)
        load_expert(0); load_expert(1)
        S.dma('sp', lambda e: e.dma_start(out=lnp[:].rearrange("p a b -> p (a b)"),
                                          in_=dr['ln1'].rearrange("a b -> (a b)").partition_broadcast(128)), 'c1', w=['lnp'])
        wrf = t("wrf", [128, 8, 36], F32, esd); wrh = t("wrh", [128, 8, 36], BF16, esd)
        S.dma('sp', lambda e: e.dma_start(out=wrf[:], in_=dr['w_r'].rearrange("(k p) f -> p k f", p=128)), 'c0', w=['wrf'])
        S.op('dve', lambda e: e.tensor_copy(out=wrh[:], in_=wrf[:]), r=['wrf'], w=['wrh'])
        trif = t("trif", [128, 128], F32, esd); trib = t("trib", [128, 128], BF16, esd)
        S.dma('sp', lambda e: e.dma_start(out=trif[:], in_=dr['c_tri'][:, :]), 'c2', w=['trif'])
        S.op('dve', lambda e: e.tensor_copy(out=trib[:], in_=trif[:]), r=['trif'], w=['trib'])
        ecap = t("ecap", [128, 32], F32, esd)
        S.dma('sp', lambda e: e.dma_start(out=ecap[:], in_=dr['c_iota'][:, 0:32]), 'c3', w=['ecap'])
        TS('dve', ecap[:], ecap[:], float(CAP), ALU.mult, ['ecap'], ['ecap'])
        cnt = t("cnt", [128, 32], F32, esd)
        S.op('dve', lambda e: e.memset(cnt[:], 0.0), w=['cnt'])
        G = 8; NHB = 12
        xs = [t(f"xsd{i}", [128, D], F32, esd) for i in range(3)]
        hbuf = [t(f"hbuf{i}", [128, D], F32, esd) for i in range(4)]
        hb = [t(f"hb{i}", [128, D], BF16, esd) for i in range(NHB)]
        hTt = [t(f"hTt{i}", [128, 8, 128], BF16, esd) for i in range(2)]
        rtb = [t(f"rtb{i}", [128, G, 36], F32, esd) for i in range(2)]
        W = t("rwork", [128, G, 96], F32, esd)
        OHa = t("OHa", [128, G, 32], F32, esd); OHb = t("OHb", [128, G, 32], F32, esd)
        val = t("valr", [128, G, 32], F32, esd); tmpv = t("tmpv", [128, G, 32], F32, esd)
        oh16 = t("oh16", [128, G, 32], BF16, esd)
        sl = t("sl", [128, G, 2], F32, esd)

        NXB = 4

        S.op('pool', lambda e: e.memset(hb[NHB - 1][:], 0.0), w=[('hb', NHB - 1)])
        S.dma('sp', lambda e: e.dma_start(out=yscr[NSLOT:NSLOT + 128, :], in_=hb[NHB - 1][:]), 'c4', r=[('hb', NHB - 1)], w=['ytrash'])

        def d_front1(ti):
            nonlocal pi
            tok = slice(ti * 128, (ti + 1) * 128)
            bi = ti % NXB
            xsi = xs[ti % 3]; xsk = ('xsd', ti % 3)
            hbf = hbuf[bi]; hk = ('hbuf', bi)
            S.dma('sp', lambda e: e.dma_start(out=xsi[:], in_=dr['x'][ti * 128:(ti + 1) * 128, :]), f'xsd{ti % 3}', w=[xsk])
            for half in range(2):
                pb = (2 * ti + half) % 6
                for k in range(8):
                    cv, ck = catv(k)
                    S.op('pe', lambda e, pb=pb, k=k, half=half, cv=cv: e.matmul(
                        ps[pb][:, :], lhsT=cv[:, tok], rhs=woutb[:, k, half * 512:(half + 1) * 512], start=(k == 0), stop=(k == 7)),
                         r=[ck, ('woutb', k)], w=[PK(pb)])
                STT(hbf[:, half * 512:(half + 1) * 512], xsi[:, half * 512:(half + 1) * 512], ALPHA, ps[pb][:, :], ALU.mult, ALU.add,
                    [xsk, PK(pb)], [hk])
            ln_stats(hbf[:], [hk], bi, part=1)

        def d_front1b(ti):
            ln_stats_b(ti % NXB, epsl)

        def d_front2(ti):
            bi = ti % NXB
            hbf = hbuf[bi]; hk = ('hbuf', bi)
            hbb = hb[ti % NHB]; hbk = ('hb', ti % NHB)
            ln_apply(hbf[:], hbf[:], [hk], [hk], bi)
            S.dma('pool', lambda e: e.dma_start(out=hscr[ti * 128:(ti + 1) * 128, :], in_=hbf[:]), f'hs{bi}', r=[hk], w=[('hscr', ti)])
            ACT(hbb[:], hbf[:], AF.Copy, [hk], [hbk])

        def d_back(ti):
            nonlocal pi
            hbb = hb[ti % NHB]; hbk = ('hb', ti % NHB)
            ht = hTt[ti % 2]; htk = ('hTt', ti % 2)
            g = ti % G; gi = ti // G
            rb = rtb[gi % 2]; rbk = ('rtb', gi % 2)
            pb = 6
            pbf = ps[pb][:, :].bitcast(BF16)
            for k in range(8):
                S.op('pe', lambda e, pbf=pbf, k=k: e.transpose(out=pbf[:, k * 128:(k + 1) * 128], in_=hbb[:, k * 128:(k + 1) * 128], identity=identb[:]),
                     r=[hbk, 'identb'], w=[PK(pb)])
            S.op('act', lambda e, pbf=pbf: e.activation(out=ht[:].rearrange("p a b -> p (a b)"), in_=pbf[:, :], func=AF.Copy), r=[PK(pb)], w=[htk])
            pb = 7
            for k in range(8):
                S.op('pe', lambda e, pb=pb, k=k: e.matmul(ps[pb][:, 0:36], lhsT=ht[:, k, :], rhs=wrh[:, k, :], start=(k == 0), stop=(k == 7)),
                     r=[htk, 'wrh'], w=[PK(pb)])
            S.op('act', lambda e, pb=pb: e.activation(out=rb[:, g, :], in_=ps[pb][:, 0:36], func=AF.Copy), r=[PK(pb)], w=[rbk])

        def bcl(v, n):
            return v.unsqueeze(2).to_broadcast([128, G, n])

        def route_group(gi):
            nonlocal pi
            rb = rtb[gi % 2]; rbk = ('rtb', gi % 2)
            t0 = gi * G
            K = ['rw']
            lg = rb[:, :, 0:4]
            mx = W[:, :, 0]; ohg = W[:, :, 1:5]; ex = W[:, :, 5:9]; gp = W[:, :, 9]
            sel = W[:, :, 10:18]; m1 = W[:, :, 18]; oh1 = W[:, :, 19:27]; sel2 = W[:, :, 27:35]; m2 = W[:, :, 35]; oh2 = W[:, :, 36:44]
            pa = W[:, :, 44]; tm8 = W[:, :, 45:53]
            S.op('dve', lambda e: e.reduce_max(out=mx, in_=lg, axis=AX.X), r=[rbk], w=K)
            TT('dve', ohg, lg, bcl(mx, 4), ALU.is_equal, [rbk] + K, K)
            TT('dve', ex, lg, bcl(mx, 4), ALU.subtract, [rbk] + K, K)
            ACT(ex, ex, AF.Exp, K, K)
            S.op('dve', lambda e: e.reduce_sum(out=gp, in_=ex, axis=AX.X), r=K, w=K)
            S.op('dve', lambda e: e.reciprocal(out=gp, in_=gp), r=K, w=K)
            TS('dve', gp, gp, 1.0 / ALPHA, ALU.mult, K, K)
            TT('dve', sel, rb[:, :, 4:12], bcl(ohg[:, :, 0], 8), ALU.mult, [rbk] + K, K)
            for g4 in range(1, 4):
                TT('dve', tm8, rb[:, :, 4 + 8 * g4:12 + 8 * g4], bcl(ohg[:, :, g4], 8), ALU.mult, [rbk] + K, K)
                TT('dve', sel, sel, tm8, ALU.add, K, K)
            S.op('dve', lambda e: e.reduce_max(out=m1, in_=sel, axis=AX.X), r=K, w=K)
            TT('dve', oh1, sel, bcl(m1, 8), ALU.is_equal, K, K)
            STT(sel2, oh1, -1e30, sel, ALU.mult, ALU.add, K, K)
            S.op('dve', lambda e: e.reduce_max(out=m2, in_=sel2, axis=AX.X), r=K, w=K)
            TT('dve', oh2, sel2, bcl(m2, 8), ALU.is_equal, K, K)
            TT('dve', pa, m2, m1, ALU.subtract, K, K)
            ACT(pa, pa, AF.Exp, K, K)
            TS('dve', pa, pa, 1.0, ALU.add, K, K)
            S.op('dve', lambda e: e.reciprocal(out=pa, in_=pa), r=K, w=K)
            wk = [('wts', t0 + g) for g in range(G)]
            TT('dve', wts[:, t0:t0 + G, 0], pa, gp, ALU.mult, K, wk)
            TT('dve', wts[:, t0:t0 + G, 1], gp, wts[:, t0:t0 + G, 0], ALU.subtract, K + wk, wk)
            O = ['ohab']
            oa4 = OHa[:].rearrange("p g (a b) -> p g a b", a=4); ob4 = OHb[:].rearrange("p g (a b) -> p g a b", a=4)
            S.op('dve', lambda e: e.tensor_copy(out=oa4, in_=oh1.unsqueeze(2).to_broadcast([128, G, 4, 8])), r=K, w=O)
            S.op('dve', lambda e: e.tensor_copy(out=ob4, in_=oh2.unsqueeze(2).to_broadcast([128, G, 4, 8])), r=K, w=O)
            TT('dve', oa4, oa4, ohg.unsqueeze(3).to_broadcast([128, G, 4, 8]), ALU.mult, K + O, O)
            TT('dve', ob4, ob4, ohg.unsqueeze(3).to_broadcast([128, G, 4, 8]), ALU.mult, K + O, O)
            TT('dve', oh16[:], OHa[:], OHb[:], ALU.add, O, ['oh16'])
            pp = 6; pt_ = 7
            for g in range(G):
                for g2 in range(g):
                    S.op('pe', lambda e, g=g, g2=g2: e.matmul(ps[pp][:, 32 * g:32 * g + 32], lhsT=onesb[:, :], rhs=oh16[:, g2, :], start=(g2 == 0), stop=False),
                         r=['onesb', 'oh16'], w=[PK(pp)])
                S.op('pe', lambda e, g=g: e.matmul(ps[pp][:, 32 * g:32 * g + 32], lhsT=trib[:, :], rhs=oh16[:, g, :], start=(g == 0), stop=True),
                     r=['trib', 'oh16'], w=[PK(pp)])
            for g in range(G):
                S.op('pe', lambda e, g=g: e.matmul(ps[pt_][:, 0:32], lhsT=onesb[:, :], rhs=oh16[:, g, :], start=(g == 0), stop=(g == G - 1)),
                     r=['onesb', 'oh16'], w=[PK(pt_)])
            Vk = ['valr']
            TT('dve', val[:], ps[pp][:, 0:32 * G].rearrange("p (g e) -> p g e", g=G), cnt[:].unsqueeze(1).to_broadcast([128, G, 32]), ALU.add, [PK(pp), 'cnt'], Vk)
            TT('dve', cnt[:], ps[pt_][:, 0:32], cnt[:], ALU.add, [PK(pt_), 'cnt'] + Vk, ['cnt'])
            TS('dve', tmpv[:], val[:], float(CAP), ALU.is_lt, Vk, Vk)
            TT('dve', val[:], val[:], ecap[:].unsqueeze(1).to_broadcast([128, G, 32]), ALU.add, Vk + ['ecap'], Vk)
            TS('dve', val[:], val[:], -float(NSLOT), ALU.add, Vk, Vk)
            TT('dve', val[:], val[:], tmpv[:], ALU.mult, Vk, Vk)
            TS('dve', val[:], val[:], float(NSLOT), ALU.add, Vk, Vk)
            TT('dve', tmpv[:], OHa[:], val[:], ALU.mult, O + Vk, Vk)
            S.op('dve', lambda e: e.reduce_sum(out=sl[:, :, 0], in_=tmpv[:], axis=AX.X), r=Vk, w=['sl'])
            TT('dve', tmpv[:], OHb[:], val[:], ALU.mult, O + Vk, Vk)
            S.op('dve', lambda e: e.reduce_sum(out=sl[:, :, 1], in_=tmpv[:], axis=AX.X), r=Vk, w=['sl'])
            sk = [('slots', t0 + g) for g in range(G)]
            S.op('dve', lambda e: e.tensor_copy(out=slots[:, 2 * t0:2 * t0 + 2 * G], in_=sl[:].rearrange("p g j -> p (g j)")), r=['sl'], w=sk)
            for g in range(G):
                ti = t0 + g
                hbb = hb[ti % NHB]; hbk = ('hb', ti % NHB)
                for j in range(2):
                    S.dma('pool', lambda e, ti=ti, j=j, hbb=hbb: e.indirect_dma_start(
                        out=xscr[:, :], out_offset=bass.IndirectOffsetOnAxis(ap=slots[:, 2 * ti + j:2 * ti + j + 1], axis=0),
                        in_=hbb[:, :], in_offset=None),
                          f'scat{ti % NHB}{j}', r=[hbk, ('slots', ti)], w=[('xscr', (ti % NHB) * 2 + j)], join=True)

        RDLY = 2
        d_front1(0); d_front1(1); d_front1(2); d_front1b(0); d_front1b(1); d_front2(0)
        for ti in range(NT):
            d_back(ti)
            if ti >= RDLY and (ti - RDLY) % G == G - 1:
                route_group((ti - RDLY) // G)
            if ti + 1 < NT:
                d_front2(ti + 1)
            if ti + 2 < NT:
                d_front1b(ti + 2)
            if ti + 3 < NT:
                d_front1(ti + 3)
        route_group(NT // G - 1)
        S.barrier()
        esd.close()
        if L['stage'] == 'D2':
            return

        ese = contextlib.ExitStack()
        NJ = CAP // 128
        Xg = [t(f"Xg{i}", [128, NJ, D], BF16, ese) for i in range(2)]
        XgT = [t(f"XgT{i}", [128, 8, CAP], BF16, ese) for i in range(2)]
        hidb = [t(f"hid{i}", [128, 4, CAP], BF16, ese) for i in range(2)]
        Yt = [t(f"Yt{i}", [128, D], BF16, ese) for i in range(3)]
        yi = 0

        def load_xg(ex_):
            wi = ex_ % 2
            S.dma('sp', lambda e: e.dma_start(out=Xg[wi][:], in_=xscr[ex_ * CAP:(ex_ + 1) * CAP, :].rearrange("(j p) d -> p j d", p=128)),
                  f'xg{wi}', r=[('xscr', i_) for i_ in range(28)], w=[('Xg', wi)])

        def e_transposes(ex_):
            nonlocal pi
            wi = ex_ % 2
            xg = Xg[wi]; xgk = ('Xg', wi); xt = XgT[wi]; xtk = ('XgT', wi)
            for j in range(NJ):
                pb = 4 + (pi % 4); pi += 1
                pbf = ps[pb][:, :].bitcast(BF16)
                for k in range(8):
                    S.op('pe', lambda e, pbf=pbf, k=k, j=j: e.transpose(out=pbf[:, k * 128:(k + 1) * 128], in_=xg[:, j, k * 128:(k + 1) * 128], identity=identb[:]),
                         r=[xgk, 'identb'], w=[PK(pb)])
                if j % 2 == 0:
                    S.op('act', lambda e, pbf=pbf, j=j: e.activation(out=xt[:, :, j * 128:(j + 1) * 128], in_=pbf[:, :].rearrange("p (a b) -> p a b", a=8), func=AF.Copy),
                         r=[PK(pb)], w=[xtk])
                else:
                    S.op('dve', lambda e, pbf=pbf, j=j: e.tensor_copy(out=xt[:, :, j * 128:(j + 1) * 128], in_=pbf[:, :].rearrange("p (a b) -> p a b", a=8)),
                         r=[PK(pb)], w=[xtk])

        def e_gateup(ex_):
            nonlocal pi
            wi = ex_ % 2
            xt = XgT[wi]; xtk = ('XgT', wi); hid = hidb[wi]; hk = ('hid', wi)
            for fc in range(4):
                pg = 4 + (pi % 4); pi += 1
                pu = 4 + (pi % 4); pi += 1
                for k in range(8):
                    S.op('pe', lambda e, pg=pg, k=k, fc=fc: e.matmul(
                        ps[pg][:, 0:CAP], lhsT=Wg[wi][:, k, fc * 128:(fc + 1) * 128], rhs=xt[:, k, :], start=(k == 0), stop=(k == 7)),
                         r=[('Wg', wi), xtk], w=[PK(pg)])
                for k in range(8):
                    S.op('pe', lambda e, pu=pu, k=k, fc=fc: e.matmul(
                        ps[pu][:, 0:CAP], lhsT=Wu[wi][:, k, fc * 128:(fc + 1) * 128], rhs=xt[:, k, :], start=(k == 0), stop=(k == 7)),
                         r=[('Wu', wi), xtk], w=[PK(pu)])
                ACT(hid[:, fc, :], ps[pg][:, 0:CAP], AF.Silu, [PK(pg)], [hk])
                TT('dve', hid[:, fc, :], hid[:, fc, :], ps[pu][:, 0:CAP], ALU.mult, [hk, PK(pu)], [hk])

        def e_down(ex_):
            nonlocal pi, yi
            wi = ex_ % 2
            hid = hidb[wi]; hk = ('hid', wi)
            for j in range(NJ):
                yt = Yt[yi % 3]; yk = ('Yt', yi % 3); ysem = f'ys{yi % 3}'; yi += 1
                for half in range(2):
                    pb = half
                    pb = (pi % 4); pi += 1
                    for fc in range(4):
                        S.op('pe', lambda e, pb=pb, fc=fc, j=j, half=half: e.matmul(
                            ps[pb][:, :], lhsT=hid[:, fc, j * 128:(j + 1) * 128], rhs=Wd[wi][:, fc, half * 512:(half + 1) * 512],
                            start=(fc == 0), stop=(fc == 3)), r=[hk, ('Wd', wi)], w=[PK(pb)])
                    if half == 0:
                        ACT(yt[:, 0:512], ps[pb][:, :], AF.Copy, [PK(pb)], [yk])
                    else:
                        S.op('dve', lambda e, yt=yt, pb=pb: e.tensor_copy(out=yt[:, 512:1024], in_=ps[pb][:, :]), r=[PK(pb)], w=[yk])
                r0 = ex_ * CAP + j * 128
                S.dma('sp', lambda e, yt=yt, r0=r0: e.dma_start(out=yscr[r0:r0 + 128, :], in_=yt[:]), ysem, r=[yk], w=[('yscr', yi % 3)], join=True)

        load_xg(0)
        load_xg(1)
        e_transposes(0)
        for ex_ in range(NEXP):
            if ex_ >= 2:
                load_expert(ex_)
            e_gateup(ex_)
            if ex_ + 1 < NEXP:
                e_transposes(ex_ + 1)
            if ex_ + 2 < NEXP:
                load_xg(ex_ + 2)
            e_down(ex_)
        S.barrier()
        ese.close()
        if L['stage'] == 'E2':
            return

        S.dma('sp', lambda e: e.dma_start(out=lnp[:].rearrange("p a b -> p (a b)"),
                                          in_=dr['ln2'].rearrange("a b -> (a b)").partition_broadcast(128)), 'c1', w=['lnp'])
        NB = 6
        Ya = [t(f"Ya{i}", [128, D], BF16) for i in range(NB)]
        Yb = [t(f"Yb{i}", [128, D], BF16) for i in range(NB)]
        hh = [t(f"hh{i}", [128, D]) for i in range(NB)]
        ykeys = [('yscr', 0), ('yscr', 1), ('yscr', 2), 'ytrash']

        def f_load(ti):
            bi = ti % NB
            ya, yb, h_ = Ya[bi], Yb[bi], hh[bi]
            S.dma('sp', lambda e: e.dma_start(out=h_[:], in_=hscr[ti * 128:(ti + 1) * 128, :]), f'hh{bi}', r=[('hscr', ti)], w=[('hh', bi)])
            S.dma('pool', lambda e: e.indirect_dma_start(
                out=ya[:, :], out_offset=None, in_=yscr[:, :], in_offset=bass.IndirectOffsetOnAxis(ap=slots[:, 2 * ti:2 * ti + 1], axis=0)),
                  f'ga{bi}', r=ykeys + [('slots', ti)], w=[('Ya', bi)])
            S.dma('pool', lambda e: e.indirect_dma_start(
                out=yb[:, :], out_offset=None, in_=yscr[:, :], in_offset=bass.IndirectOffsetOnAxis(ap=slots[:, 2 * ti + 1:2 * ti + 2], axis=0)),
                  f'gb{bi}', r=ykeys + [('slots', ti)], w=[('Yb', bi)])

        epsl2 = t("epsl2", [128, 1])
        S.op('dve', lambda e: e.memset(epsl2[:], LN_EPS / (ALPHA * ALPHA)), w=['epsl'])

        def f_c1a(ti):
            bi = ti % NB
            ya, yb, h_ = Ya[bi], Yb[bi], hh[bi]
            STT(h_[:], ya[:], wts[:, ti, 0:1], h_[:], ALU.mult, ALU.add, [('Ya', bi), ('hh', bi), ('wts', ti)], [('hh', bi)])
            STT(h_[:], yb[:], wts[:, ti, 1:2], h_[:], ALU.mult, ALU.add, [('Yb', bi), ('hh', bi), ('wts', ti)], [('hh', bi)])
            ln_stats(h_[:], [('hh', bi)], bi, part=1)

        def f_c1b(ti):
            ln_stats_b(ti % NB, epsl2)

        def f_c2a(ti):
            bi = ti % NB
            ln_apply(hh[bi][:], hh[bi][:], [('hh', bi)], [('hh', bi)], bi, part='a')

        def f_c2b(ti):
            bi = ti % NB
            h_ = hh[bi]
            ln_apply(h_[:], h_[:], [('hh', bi)], [('hh', bi)], bi, part='b')
            S.dma('sp', lambda e: e.dma_start(out=y[ti * 128:(ti + 1) * 128, :], in_=h_[:]), f'yo{bi}',
                  r=[('hh', bi)], w=[('y', ti), ('hh', bi)])

        f_load(0); f_load(1); f_load(2); f_load(3)
        f_c1a(0); f_c1b(0); f_c1a(1)
        for ti in range(NT):
            f_c2a(ti)
            if ti + 2 < NT:
                f_c1a(ti + 2)
            if ti + 1 < NT:
                f_c1b(ti + 1)
            f_c2b(ti)
            if ti + 4 < NT:
                f_load(ti + 4)
        S.finish([('y', ti) for ti in range(NT)])


def _consts():
    ident = np.eye(128, dtype=np.float32)
    tri = np.triu(np.ones((128, 128), np.float32), 1)
    iota = np.tile(np.arange(64, dtype=np.float32)[None, :], (128, 1))
    return ident, tri, iota


def make_in_maps(inp):
    ident, tri, iota = _consts()
    sq = lambda a: np.ascontiguousarray(a[0])
    shared = {
        "w_in": sq(inp["w_in"]),
        "lamv": np.ascontiguousarray(np.stack([inp["lam_q1"][0], inp["lam_k1"][0], inp["lam_q2"][0], inp["lam_k2"][0]])),
        "subln_g": sq(inp["subln_g"]),
        "ssm_a_re": sq(inp["ssm_a_re"]), "ssm_a_im": sq(inp["ssm_a_im"]), "ssm_log_dt": sq(inp["ssm_log_dt"]),
        "ssm_b_re": sq(inp["ssm_b_re"]), "ssm_b_im": sq(inp["ssm_b_im"]),
        "ssm_c_re": sq(inp["ssm_c_re"]), "ssm_c_im": sq(inp["ssm_c_im"]),
        "ssm_d": np.ascontiguousarray(inp["ssm_d"][0].reshape(512)), "w_glu": sq(inp["w_glu"]), "b_glu": sq(inp["b_glu"]),
        "w_out": sq(inp["w_out"]),
        "ln1": np.ascontiguousarray(np.stack([inp["ln1_g"][0], inp["ln1_b"][0]])),
        "ln2": np.ascontiguousarray(np.stack([inp["ln2_g"][0], inp["ln2_b"][0]])),
        "w_r": np.ascontiguousarray(np.concatenate([inp["w_router_group"][0], inp["w_router_expert"][0]], axis=1)),
        "w_exp_gate": sq(inp["w_exp_gate"]), "w_exp_up": sq(inp["w_exp_up"]), "w_exp_down": sq(inp["w_exp_down"]),
        "c_ident": ident, "c_tri": tri, "c_iota": iota,
    }
    def lay_gp(a):
        r = a.reshape((16, 2, 64) + a.shape[2:])
        r = np.moveaxis(r, 0, 2)
        return np.ascontiguousarray(r.reshape((128, 16) + a.shape[2:]))
    bd = np.kron(np.eye(4, dtype=np.float32), np.ones((32, 32), np.float32))
    rm = np.kron(np.eye(4, dtype=np.float32), np.ones((32, 1), np.float32))
    shared.update({
        "c_bdmask": bd, "c_rmask": rm,
        "s_are": lay_gp(inp["ssm_a_re"][0]), "s_aim": lay_gp(inp["ssm_a_im"][0]),
        "s_ldt": lay_gp(np.repeat(inp["ssm_log_dt"][0][:, None], 64, axis=1)),
        "s_bre": lay_gp(inp["ssm_b_re"][0]).reshape(128, 256), "s_bim": lay_gp(inp["ssm_b_im"][0]).reshape(128, 256),
        "s_cre": lay_gp(np.transpose(inp["ssm_c_re"][0], (0, 2, 1))).reshape(128, 256),
        "s_cim": lay_gp(np.transpose(inp["ssm_c_im"][0], (0, 2, 1))).reshape(128, 256),
        "s_d": np.ascontiguousarray(inp["ssm_d"][0].reshape(4, 128).T), "s_bglu": np.ascontiguousarray(inp["b_glu"][0].reshape(4, 128).T),
    })
    return shared


def kernel(**inp):
    inp = {k: np.asarray(v) for k, v in inp.items()}
    stage = os.environ.get("MK_STAGE", "F")
    ncores = int(os.environ.get("MK_CORES", "8"))
    nc = build_program(stage)
    shared = make_in_maps(inp)
    in_maps = []
    for c in range(ncores):
        m = dict(shared); m["x"] = np.ascontiguousarray(inp["x"][c]); in_maps.append(m)
    res = run_bass_kernel_spmd(nc, in_maps, core_ids=list(range(ncores)))
    if stage == 'E':
        return [r["y"] for r in res.results]
    if stage != 'F':
        return [r["dbg"] for r in res.results]
    return np.stack([r["y"] for r in res.results], axis=0).astype(np.float32)
```
